# Optimizing a Trainium2 kernel written in Bass

```python
import math
import jax, jax.numpy as jnp
from jax import lax
import numpy as np

D_MODEL = 1024
BATCH = 16
SEQ = 2048
DEPTH = 1

PLE_DIM = 256
GRID_W = 64
MIX_WIDTH = D_MODEL
HY_WIDTH = MIX_WIDTH // 2
NA_WIDTH = MIX_WIDTH - HY_WIDTH
NA_HEADS = 8
NA_HEAD_DIM = NA_WIDTH // NA_HEADS
HY_ORDER = 2
SHORT_CONV = 3
FILTER_EMB = 33
FILTER_BANDS = (FILTER_EMB - 1) // 2
FILTER_HIDDEN = 64
DECAY_TARGET = 1e-2
FAST_DECAY_PCT = 0.3
SLOW_DECAY_PCT = 1.5
WIN_ROWS = 8
WIN_COLS = 16
Q_ROWS = 2
Q_COLS = 16
N_GROUPS = 4
EXPERTS_PER_GROUP = 8
N_EXPERTS = N_GROUPS * EXPERTS_PER_GROUP
TOP_K = 2
D_EXPERT = D_MODEL // 2
MOE_BLOCK = 256
EPS = 1e-6
NEG_INF = -1e30
HY_COLS = (HY_ORDER + 1) * HY_WIDTH
IN_COLS = HY_COLS + 3 * NA_WIDTH

kernel_name = 'hybrid_hyena_natten_hiermoe_block'


def _rmsnorm(x, g):
    xf = x.astype(jnp.float32)
    y = xf * lax.rsqrt(jnp.mean(xf * xf, axis=-1, keepdims=True) + EPS)
    return (y * g.astype(jnp.float32)).astype(x.dtype)


def _short_conv(u, w, b):
    L = u.shape[1]
    pad = SHORT_CONV // 2
    up = jnp.pad(u, ((0, 0), (pad, SHORT_CONV - 1 - pad), (0, 0)))
    out = b
    for j in range(SHORT_CONV):
        out = out + up[:, j:j + L] * w[j]
    return out


def _hyena_filters(L, w1, b1, f1, w2, b2, f2, w3):
    f32 = jnp.float32
    t = jnp.linspace(0.0, 1.0, L, dtype=f32)[:, None]
    w = 2.0 * math.pi * jnp.arange(L, dtype=f32)[:, None] / L
    bands = jnp.linspace(1e-4, FILTER_BANDS - 1, FILTER_BANDS, dtype=f32)[None, :]
    z = jnp.concatenate([t, jnp.cos(bands * w), -jnp.sin(bands * w)], axis=-1)
    hid = jnp.sin(f1.astype(f32) * (z @ w1.astype(f32) + b1.astype(f32)))
    hid = jnp.sin(f2.astype(f32) * (hid @ w2.astype(f32) + b2.astype(f32)))
    hf = (hid @ w3.astype(f32)).reshape(L, 2, HY_ORDER, HY_WIDTH)
    max_decay = math.log(DECAY_TARGET) / FAST_DECAY_PCT
    min_decay = math.log(DECAY_TARGET) / SLOW_DECAY_PCT
    deltas = jnp.linspace(min_decay, max_decay, HY_WIDTH, dtype=f32)
    decay = jnp.exp(-t * jnp.abs(deltas)[None, :])
    hf = hf * decay[:, None, None, :]
    fwd, bwd = hf[:, 0], hf[:, 1]
    k = jnp.concatenate([fwd, jnp.zeros_like(fwd[:1]), bwd[:0:-1]], axis=0)
    return jnp.fft.rfft(k, axis=0)


def _hyena(u, conv_w, conv_b, filt_fft, skip):
    L = u.shape[1]
    u = _short_conv(u, conv_w, conv_b).astype(jnp.float32)
    x1, x2, v = jnp.split(u, 3, axis=-1)
    z = v
    for o, gate in enumerate((x1, x2)):
        zf = jnp.fft.rfft(z, n=2 * L, axis=1)
        conv = jnp.fft.irfft(zf * filt_fft[:, o][None], n=2 * L, axis=1)[:, :L]
        z = gate * (conv + z * skip[o].astype(jnp.float32))
    return z


def _natten_tables(rows):
    kr = min(WIN_ROWS, rows)
    krb = min(Q_ROWS + kr - 1, rows)
    kcb = min(Q_COLS + WIN_COLS - 1, GRID_W)
    npr, ncb = rows // Q_ROWS, GRID_W // Q_COLS
    rs = np.clip(np.arange(rows) - kr // 2, 0, rows - kr)
    cs = np.clip(np.arange(GRID_W) - WIN_COLS // 2, 0, GRID_W - WIN_COLS)
    q_r = np.arange(npr)[:, None] * Q_ROWS + np.arange(Q_ROWS)
    q_c = np.arange(ncb)[:, None] * Q_COLS + np.arange(Q_COLS)
    k_r = np.minimum(rs[q_r[:, 0]], rows - krb)[:, None] + np.arange(krb)
    k_c = np.minimum(cs[q_c[:, 0]], GRID_W - kcb)[:, None] + np.arange(kcb)
    nq, nk = Q_ROWS * Q_COLS, krb * kcb
    qr = np.broadcast_to(q_r[:, None, :, None], (npr, ncb, Q_ROWS, Q_COLS)).reshape(npr, ncb, nq, 1)
    qc = np.broadcast_to(q_c[None, :, None, :], (npr, ncb, Q_ROWS, Q_COLS)).reshape(npr, ncb, nq, 1)
    kr_ = np.broadcast_to(k_r[:, None, :, None], (npr, ncb, krb, kcb)).reshape(npr, ncb, 1, nk)
    kc_ = np.broadcast_to(k_c[None, :, None, :], (npr, ncb, krb, kcb)).reshape(npr, ncb, 1, nk)
    key_idx = (kr_ * GRID_W + kc_)[:, :, 0, :].astype(np.int32)
    valid = ((kr_ >= rs[qr]) & (kr_ < rs[qr] + kr)
             & (kc_ >= cs[qc]) & (kc_ < cs[qc] + WIN_COLS))
    dr = np.clip(kr_ - qr + WIN_ROWS - 1, 0, 2 * WIN_ROWS - 2).astype(np.int32)
    dc = np.clip(kc_ - qc + WIN_COLS - 1, 0, 2 * WIN_COLS - 2).astype(np.int32)
    return key_idx, valid, dr, dc


def _natten(qkv, rpb):
    B, L, _ = qkv.shape
    rows = L // GRID_W
    npr, ncb = rows // Q_ROWS, GRID_W // Q_COLS
    key_idx, valid, dr, dc = _natten_tables(rows)
    q, k, v = jnp.split(qkv, 3, axis=-1)
    q = q.reshape(B, npr, Q_ROWS, ncb, Q_COLS, NA_HEADS, NA_HEAD_DIM)
    q = q.transpose(0, 1, 3, 2, 4, 5, 6).reshape(B, npr, ncb, Q_ROWS * Q_COLS, NA_HEADS, NA_HEAD_DIM)
    k = k.reshape(B, L, NA_HEADS, NA_HEAD_DIM)
    v = v.reshape(B, L, NA_HEADS, NA_HEAD_DIM)
    kb = jnp.take(k, key_idx, axis=1)
    vb = jnp.take(v, key_idx, axis=1)
    s = jnp.einsum('bpjqhd,bpjkhd->bhpjqk', q, kb).astype(jnp.float32) * (NA_HEAD_DIM ** -0.5)
    bias = rpb.astype(jnp.float32)[:, dr, dc]
    s = jnp.where(valid, s + bias, NEG_INF)
    a = jax.nn.softmax(s, axis=-1).astype(vb.dtype)
    o = jnp.einsum('bhpjqk,bpjkhd->bpjqhd', a, vb)
    o = o.reshape(B, npr, ncb, Q_ROWS, Q_COLS, NA_WIDTH).transpose(0, 1, 3, 2, 4, 5)
    return o.reshape(B, L, NA_WIDTH)


def _moe(x, wg, bg, we, be, w1, w3, w2):
    B, L, D = x.shape
    T = B * L
    f32 = jnp.float32
    xf = x.reshape(T, D)
    xr = xf.astype(f32)
    g_prob = jax.nn.softmax(xr @ wg.astype(f32) + bg.astype(f32), axis=-1)
    g_w, g_idx = lax.top_k(g_prob, 1)
    e_logits = (xr @ we.astype(f32) + be.astype(f32)).reshape(T, N_GROUPS, EXPERTS_PER_GROUP)
    e_logits = jnp.take_along_axis(e_logits, g_idx[:, :, None], axis=1)[:, 0]
    e_w, e_idx = lax.top_k(jax.nn.softmax(e_logits, axis=-1), TOP_K)
    gates = g_w * (e_w / jnp.sum(e_w, axis=-1, keepdims=True))
    ids = (g_idx * EXPERTS_PER_GROUP + e_idx).astype(jnp.int32)
    A = T * TOP_K
    flat = ids.reshape(-1)
    order = jnp.argsort(flat)
    sid = flat[order]
    tok = (order // TOP_K).astype(jnp.int32)
    gate_s = gates.reshape(-1)[order]
    counts = jnp.bincount(flat, length=N_EXPERTS)
    padded = (counts + MOE_BLOCK - 1) // MOE_BLOCK * MOE_BLOCK
    pad_end = jnp.cumsum(padded)
    pad_start = pad_end - padded
    seg_start = jnp.cumsum(counts) - counts
    dest = pad_start[sid] + jnp.arange(A, dtype=jnp.int32) - seg_start[sid]
    n_blocks = -(-(A + N_EXPERTS * (MOE_BLOCK - 1)) // MOE_BLOCK)
    slot_tok = jnp.full((n_blocks * MOE_BLOCK,), T, jnp.int32).at[dest].set(tok)
    block_e = jnp.minimum(jnp.searchsorted(pad_end, jnp.arange(n_blocks) * MOE_BLOCK, side='right'), N_EXPERTS - 1)
    xpad = jnp.concatenate([xf, jnp.zeros((1, D), xf.dtype)], axis=0)
    xb = xpad[slot_tok].reshape(n_blocks, MOE_BLOCK, D)

    def expert_block(args):
        xblk, e = args
        hid = jax.nn.silu(xblk @ w1[e]) * (xblk @ w3[e])
        return hid @ w2[e]

    yb = lax.map(expert_block, (xb, block_e)).reshape(-1, D)
    y = jnp.zeros((T, D), x.dtype).at[tok].add(yb[dest] * gate_s[:, None].astype(x.dtype))
    return y.reshape(B, L, D)


def setup_inputs(seed: int = 0) -> dict:
    key = jax.random.key(seed)
    ks = jax.random.split(key, 32)
    n = jax.random.normal
    f32 = jnp.float32

    def gain(k, shape):
        return 1.0 + 0.01 * n(k, shape, f32)

    return {
        'x': n(ks[0], (BATCH, SEQ, D_MODEL), f32),
        'p': n(ks[1], (DEPTH, BATCH, SEQ, PLE_DIM), f32),
        'g_mix': gain(ks[2], (DEPTH, D_MODEL)),
        'w_in': n(ks[3], (DEPTH, D_MODEL, IN_COLS), f32) * D_MODEL ** -0.5,
        'hy_conv_w': n(ks[4], (DEPTH, SHORT_CONV, HY_COLS), f32) * SHORT_CONV ** -0.5,
        'hy_conv_b': 0.01 * n(ks[5], (DEPTH, HY_COLS), f32),
        'hy_f_w1': n(ks[6], (DEPTH, FILTER_EMB, FILTER_HIDDEN), f32) * FILTER_EMB ** -0.5,
        'hy_f_b1': 0.01 * n(ks[7], (DEPTH, FILTER_HIDDEN), f32),
        'hy_f_freq1': gain(ks[8], (DEPTH, FILTER_HIDDEN)),
        'hy_f_w2': n(ks[9], (DEPTH, FILTER_HIDDEN, FILTER_HIDDEN), f32) * FILTER_HIDDEN ** -0.5,
        'hy_f_b2': 0.01 * n(ks[10], (DEPTH, FILTER_HIDDEN), f32),
        'hy_f_freq2': gain(ks[11], (DEPTH, FILTER_HIDDEN)),
        'hy_f_w3': n(ks[12], (DEPTH, FILTER_HIDDEN, 2 * HY_ORDER * HY_WIDTH), f32) * (0.1 * FILTER_HIDDEN ** -0.5),
        'hy_skip': n(ks[13], (DEPTH, HY_ORDER, HY_WIDTH), f32),
        'na_rpb': 0.02 * n(ks[14], (DEPTH, NA_HEADS, 2 * WIN_ROWS - 1, 2 * WIN_COLS - 1), f32),
        'g_out_hy': gain(ks[15], (DEPTH, HY_WIDTH)),
        'g_out_na': gain(ks[16], (DEPTH, NA_WIDTH)),
        'w_out': n(ks[17], (DEPTH, MIX_WIDTH, D_MODEL), f32) * MIX_WIDTH ** -0.5,
        'g_ffn': gain(ks[18], (DEPTH, D_MODEL)),
        'router_wg': n(ks[19], (DEPTH, D_MODEL, N_GROUPS), f32) * D_MODEL ** -0.5,
        'router_bg': 0.01 * n(ks[20], (DEPTH, N_GROUPS), f32),
        'router_we': n(ks[21], (DEPTH, D_MODEL, N_EXPERTS), f32) * D_MODEL ** -0.5,
        'router_be': 0.01 * n(ks[22], (DEPTH, N_EXPERTS), f32),
        'exp_w1': n(ks[23], (DEPTH, N_EXPERTS, D_MODEL, D_EXPERT), f32) * D_MODEL ** -0.5,
        'exp_w3': n(ks[24], (DEPTH, N_EXPERTS, D_MODEL, D_EXPERT), f32) * D_MODEL ** -0.5,
        'exp_w2': n(ks[25], (DEPTH, N_EXPERTS, D_EXPERT, D_MODEL), f32) * D_EXPERT ** -0.5,
        'g_ple': gain(ks[26], (DEPTH, D_MODEL)),
        'w_ple_gate': n(ks[27], (DEPTH, D_MODEL, D_MODEL), f32) * D_MODEL ** -0.5,
        'w_ple_proj': n(ks[28], (DEPTH, PLE_DIM, D_MODEL), f32) * PLE_DIM ** -0.5,
        'g_final': gain(ks[29], (D_MODEL,)),
    }


def reference(x, p, g_mix, w_in, hy_conv_w, hy_conv_b, hy_f_w1, hy_f_b1, hy_f_freq1, hy_f_w2, hy_f_b2,
              hy_f_freq2, hy_f_w3, hy_skip, na_rpb, g_out_hy, g_out_na, w_out, g_ffn, router_wg, router_bg,
              router_we, router_be, exp_w1, exp_w3, exp_w2, g_ple, w_ple_gate, w_ple_proj, g_final):
    L = x.shape[1]
    h = x
    for i in range(DEPTH):
        u = _rmsnorm(h, g_mix[i]) @ w_in[i]
        filt = _hyena_filters(L, hy_f_w1[i], hy_f_b1[i], hy_f_freq1[i], hy_f_w2[i], hy_f_b2[i],
                              hy_f_freq2[i], hy_f_w3[i])
        y_hy = _hyena(u[..., :HY_COLS], hy_conv_w[i], hy_conv_b[i], filt, hy_skip[i])
        y_na = _natten(u[..., HY_COLS:], na_rpb[i])
        mixed = jnp.concatenate([_rmsnorm(y_hy, g_out_hy[i]).astype(h.dtype),
                                 _rmsnorm(y_na, g_out_na[i]).astype(h.dtype)], axis=-1)
        h = h + mixed @ w_out[i]
        h = h + _moe(_rmsnorm(h, g_ffn[i]), router_wg[i], router_bg[i], router_we[i], router_be[i],
                     exp_w1[i], exp_w3[i], exp_w2[i])
        gate = jax.nn.sigmoid(_rmsnorm(h, g_ple[i]) @ w_ple_gate[i])
        h = h + (p[i] @ w_ple_proj[i]) * gate
    return _rmsnorm(h, g_final)
```

```python
import math
from contextlib import ExitStack

import numpy as np
import ml_dtypes

import concourse.bass as bass
import concourse.mybir as mybir
from concourse.bass_utils import run_bass_kernel_spmd

F32 = mybir.dt.float32
BF16 = mybir.dt.bfloat16
I32 = mybir.dt.int32
U32 = mybir.dt.uint32
AF = mybir.ActivationFunctionType
ALU = mybir.AluOpType
AX = mybir.AxisListType

NCORES = 8
D = 1024
L = 2048
NSEQ = 2
T = NSEQ * L
NT = T // 128
HYW = 512
HYC = 1536
NAH = 8
NE = 32
CAP = 512
DEXP = 512
PLE = 256
EPS = 1e-6
XW = L + 2


class Buf:
    __slots__ = ("name", "w", "r")

    def __init__(self, name):
        self.name = name
        self.w = None
        self.r = []


def bufs(prefix, n):
    return [Buf(f"{prefix}{i}") for i in range(n)]


class Sync:
    EPOCH = 8192
    NDSEM = 8

    def __init__(self, nc, es):
        self.nc = nc
        self.es = es
        self.eng = {"pe": nc.tensor, "act": nc.scalar, "dve": nc.vector, "pool": nc.gpsimd, "sp": nc.sync}
        self.cnt = {e: 0 for e in self.eng}
        self.esems = {e: [] for e in self.eng}
        self.seen = {e: {} for e in self.eng}
        self.dsems = {}
        self.dk = {}
        self.nsem = 0

    def _newsem(self, name):
        self.nsem += 1
        return self.es.enter_context(self.nc.semaphore(name))

    def _esem(self, e, epoch):
        lst = self.esems[e]
        while len(lst) <= epoch:
            lst.append(self._newsem(f"s_{e}_{len(lst)}"))
        return lst[epoch]

    def _wait(self, E, tok):
        if tok is None:
            return
        kind = tok[0]
        if kind == "c":
            _, e, c = tok
            if e == "pe" and E == "pe":
                return
            if self.seen[E].get(e, 0) >= c:
                return
            epoch = (c - 1) // self.EPOCH
            val = (c - 1) % self.EPOCH + 1
            self.eng[E].wait_ge(self._esem(e, epoch), val)
            self.seen[E][e] = c
        else:
            _, key, val = tok
            if self.seen[E].get(key, 0) >= val:
                return
            self.eng[E].wait_ge(self.dsems[key], val)
            self.seen[E][key] = val

    def _deps(self, E, reads, writes):
        toks = []
        for b in reads:
            if b.w is not None:
                toks.append(b.w)
        for b in writes:
            if b.w is not None:
                toks.append(b.w)
            toks.extend(b.r)
        best = {}
        for t in toks:
            k = t[1]
            if k not in best or best[k][2] < t[2]:
                best[k] = t
        for t in best.values():
            self._wait(E, t)

    def _mark(self, tok, reads, writes):
        for b in reads:
            if tok[0] == "c":
                b.r = [t for t in b.r if not (t[0] == "c" and t[1] == tok[1])]
            b.r.append(tok)
        for b in writes:
            b.w = tok
            b.r = []

    def op(self, E, fn, reads=(), writes=()):
        self._deps(E, reads, writes)
        ins = fn(self.eng[E])
        self.cnt[E] += 1
        c = self.cnt[E]
        ins.then_inc(self._esem(E, (c - 1) // self.EPOCH), 1)
        tok = ("c", E, c)
        self._mark(tok, reads, writes)
        return tok

    def chain(self, E, fns, reads=(), writes=()):
        link = Buf("chain")
        tok = None
        for fn in fns:
            tok = self.op(E, fn, reads=list(reads) + [link], writes=list(writes) + [link])
        return tok

    def dma(self, Q, fn, reads=(), writes=()):
        k = self.dk.get(Q, 0)
        self.dk[Q] = k + 1
        idx = k % self.NDSEM
        key = (Q, idx)
        if key not in self.dsems:
            self.dsems[key] = self._newsem(f"d_{Q}_{idx}")
        rnd = k // self.NDSEM
        if rnd > 0:
            self._wait(Q, ("d", key, 16 * rnd))
        self._deps(Q, reads, writes)
        ins = fn(self.eng[Q])
        ins.then_inc(self.dsems[key], 16)
        tok = ("d", key, 16 * (rnd + 1))
        self._mark(tok, reads, writes)
        return tok

    def barrier(self):
        for E in self.eng:
            for e in self.eng:
                if e != E and self.cnt[e] > 0:
                    self._wait(E, ("c", e, self.cnt[e]))
            for key in self.dsems:
                q, idx = key
                k = self.dk.get(q, 0)
                n = (k - idx + self.NDSEM - 1) // self.NDSEM
                if n > 0:
                    self._wait(E, ("d", key, 16 * n))

    def finish(self, E, bufs_):
        for b in bufs_:
            self._wait(E, b.w)


def _consts():
    c = {}
    c["ident_bf"] = np.eye(128, dtype=np.float32).astype(ml_dtypes.bfloat16)
    c["ident_f"] = np.eye(128, dtype=np.float32)
    t = np.linspace(0.0, 1.0, L, dtype=np.float32)[:, None]
    w = (2.0 * math.pi * np.arange(L, dtype=np.float32)[:, None] / L).astype(np.float32)
    bands = np.linspace(1e-4, 15.0, 16, dtype=np.float32)[None, :]
    z = np.concatenate([t, np.cos(bands * w), -np.sin(bands * w)], axis=-1).astype(np.float32)
    c["zT"] = np.ascontiguousarray(z.T)
    max_decay = math.log(1e-2) / 0.3
    min_decay = math.log(1e-2) / 1.5
    deltas = np.linspace(min_decay, max_decay, HYW, dtype=np.float32)
    decay = np.exp(-t * np.abs(deltas)[None, :]).astype(np.float32)
    c["dec_f"] = decay
    db = decay.copy()
    db[0, :] = 0.0
    c["dec_b"] = db
    N = 2 * L
    tt = np.arange(L, dtype=np.float64)
    om = (np.arange(L, dtype=np.float64) + 0.5) * (2.0 * math.pi / N)
    ang = np.outer(tt, om)
    Cf = np.cos(ang)
    Sf = np.sin(ang)

    def fwd_layout(M):
        return np.ascontiguousarray(M.reshape(16, 128, 16, 128).transpose(2, 1, 0, 3)).reshape(16, 128, 2048)

    def inv_layout(M):
        return np.ascontiguousarray(M.reshape(16, 128, 16, 128).transpose(0, 3, 2, 1)).reshape(16, 128, 2048)
    bf = ml_dtypes.bfloat16
    c["Cf"] = fwd_layout(Cf).astype(np.float32).astype(bf)
    c["Sf"] = fwd_layout(Sf).astype(np.float32).astype(bf)
    c["Ci"] = inv_layout(Cf * (2.0 / N)).astype(np.float32).astype(bf)
    c["Si"] = inv_layout(Sf * (2.0 / N)).astype(np.float32).astype(bf)
    return c


def _natten_consts():
    rows, GW, WR, WC = 32, 64, 8, 16
    rs = np.clip(np.arange(rows) - WR // 2, 0, rows - WR)
    cs = np.clip(np.arange(GW) - WC // 2, 0, GW - WC)
    masks = {}
    for p in range(16):
        P0 = min(max(p - 2, 0), 11)
        m = np.zeros((128, 5, 128), np.float32)
        ak = np.arange(128) // 64
        kc = np.arange(128) % 64
        a = np.arange(128) // 64
        qc = np.arange(128) % 64
        for jp in range(5):
            kr = 2 * (P0 + 4 - jp) + ak
            qr = 2 * p + a
            rv = (kr[:, None] >= rs[qr][None, :]) & (kr[:, None] < rs[qr][None, :] + WR)
            cv = (kc[:, None] >= cs[qc][None, :]) & (kc[:, None] < cs[qc][None, :] + WC)
            m[:, jp, :] = np.where(rv & cv, 0.0, -30000.0)
        masks[p] = m.reshape(128, 640)
    types = [0, 1, 2, 14, 15]
    for p in range(2, 14):
        assert np.array_equal(masks[p], masks[2])
    return np.ascontiguousarray(np.stack([masks[t] for t in types], axis=1))


def _rpb_toeplitz(rpb):
    H = rpb.shape[0]
    pad = np.zeros((H, 15 + 8, 31 + 128), np.float32)
    pad[:, 4:4 + 15, 64:64 + 31] = rpb
    ak = (np.arange(128) // 64)[:, None, None]
    kc = (np.arange(128) % 64)[:, None, None]
    e = np.arange(18)[None, :, None]
    qc = np.arange(64)[None, None, :]
    dr = 15 + ak - e + np.zeros_like(qc)
    dc = kc - qc + 15 + np.zeros_like(e)
    dr, dc = np.broadcast_arrays(dr, dc)
    out = pad[:, dr + 4, dc + 64]
    return np.ascontiguousarray(out.reshape(H, 128, 18 * 64))


def _pc(v, nchunk):
    return np.ascontiguousarray(np.asarray(v).reshape(nchunk, 128).T)


def build_nc(debug=False, stop_after=None):
    nc = bass.Bass("TRN2", target_bir_lowering=False)
    es = ExitStack()
    S = Sync(nc, es)

    def din(name, shape, dt=F32):
        return nc.dram_tensor(name, list(shape), dt, kind="ExternalInput").ap()

    def dscratch(name, shape, dt=F32):
        kind = "ExternalOutput" if debug else "Internal"
        return nc.dram_tensor(name, list(shape), dt, kind=kind).ap()

    def sb(stack, name, shape, dt):
        return stack.enter_context(nc.sbuf_tensor("sb_" + name, list(shape), dt))

    def ps(stack, name, shape, dt):
        return stack.enter_context(nc.psum_tensor("ps_" + name, list(shape), dt))

    def pipeline(n, stages, skews):
        for s_ in range(n + max(skews)):
            for st_, sk_ in zip(stages, skews):
                jj = s_ - sk_
                if 0 <= jj < n:
                    st_(jj)

    def rsqrt_dve(st6, scale, bl):
        a, y, t_, u_ = st6[:, 1:2], st6[:, 2:3], st6[:, 3:4], st6[:, 4:5]
        fns = [
            lambda e: e.tensor_scalar(out=a, in0=st6[:, 0:1], scalar1=scale, scalar2=EPS, op0=ALU.mult, op1=ALU.add),
            lambda e: e.tensor_scalar(out=y.bitcast(I32), in0=a.bitcast(I32), scalar1=-0.5, scalar2=1597463007.0,
                                      op0=ALU.mult, op1=ALU.add),
        ]
        for _ in range(3):
            fns += [
                lambda e: e.scalar_tensor_tensor(out=t_, in0=y, scalar=a, in1=y, op0=ALU.mult, op1=ALU.mult),
                lambda e: e.tensor_scalar(out=u_, in0=t_, scalar1=-0.5, scalar2=1.5, op0=ALU.mult, op1=ALU.add),
                lambda e: e.tensor_tensor(out=y, in0=y, in1=u_, op=ALU.mult),
            ]
        S.chain("dve", fns, reads=bl, writes=bl)

    def dbg(name, ap, shape, dt, reads):
        if not debug:
            return
        dtn = nc.dram_tensor("dbg_" + name, list(shape), dt, kind="ExternalOutput").ap()
        bb = Buf("dbg_" + name)
        S.dma("sp", lambda e: e.dma_start(out=dtn, in_=ap), reads=reads, writes=[bb])
        out_bufs.append(bb)

    out_bufs = []
    x = din("x", [T, D])
    w_in = din("w_in", [D, 3072])
    gmix_pc = din("gmix_pc", [128, 8])
    conv_w = din("conv_w", [3, HYC])
    conv_b = din("conv_b", [1, HYC])
    ident_bf_d = din("ident_bf", [128, 128], BF16)
    ident_f_d = din("ident_f", [128, 128])
    zT_d = din("zT", [33, L])
    fw1_d = din("fw1", [33, 64])
    fw2_d = din("fw2", [64, 64])
    fw3_d = din("fw3", [64, 2048])
    fcol_d = din("fcol", [64, 4])
    decf_d = din("dec_f", [L, HYW])
    decb_d = din("dec_b", [L, HYW])
    Cf_d = din("Cf", [16, 128, 2048], BF16)
    Sf_d = din("Sf", [16, 128, 2048], BF16)
    Ci_d = din("Ci", [16, 128, 2048], BF16)
    Si_d = din("Si", [16, 128, 2048], BF16)
    skip_d = din("skip", [1, 2 * HYW])
    ghy_d = din("ghy", [1, HYW])
    gna_d = din("gna", [1, HYW])
    w_out_d = din("w_out", [D, D])
    gffn_d = din("gffn", [1, D])
    wr_d = din("wr_pc", [128, 8, 36])
    br_d = din("br", [1, 36])
    ust_d = din("ust", [128, 128])
    onesf_d = din("ones_f", [128, 128])
    ecap_d = din("ecap", [1, NE])
    ew1_d = din("exp_w1", [NE, D, DEXP])
    ew3_d = din("exp_w3", [NE, D, DEXP])
    ew2_d = din("exp_w2", [NE, DEXP, D])
    gple_d = din("gple_pc", [128, 8])
    wgate_d = din("w_gate", [D, D])
    wproj_d = din("w_proj", [PLE, D])
    gfin_d = din("gfin", [1, D])
    p_d = din("p", [T, PLE])
    nmask_d = din("nmask", [128, 5 * 640])
    rpbT_d = din("rpbT", [NAH, 128, 18 * 64])
    out = nc.dram_tensor("out", [T, D], F32, kind="ExternalOutput").ap()

    U_hy = dscratch("U_hy", [T, HYC])
    QT = dscratch("QT", [4, 128, T], BF16)
    KT = dscratch("KT", [4, 128, T], BF16)
    VA = dscratch("VA", [T, NAH * 65], BF16)
    Kscr = dscratch("Kscr", [2, 16, 128, 2 * HYW])
    Z1 = dscratch("Z1", [T, HYW])
    MIX = dscratch("MIX", [T, D], BF16)
    H1 = dscratch("H1", [T, D])
    XS = dscratch("XS", [NE * CAP, D], BF16)
    YS = dscratch("YS", [NE * CAP, D])

    gs = es
    ident_bf = sb(gs, "ident_bf_s", [128, 128], BF16)
    ident_f = sb(gs, "ident_f_s", [128, 128], F32)
    b_ident = Buf("ident")
    S.dma("sp", lambda e: e.dma_start(out=ident_bf[:], in_=ident_bf_d[:, :]), writes=[b_ident])
    S.dma("sp", lambda e: e.dma_start(out=ident_f[:], in_=ident_f_d[:, :]), writes=[b_ident])

    desti = sb(gs, "desti", [128, NT, 2], I32)
    gates = sb(gs, "gates", [128, NT, 2], F32)
    b_route = bufs("route", NT)

    with ExitStack() as pa:
        pa.enter_context(nc.named_scope("phA"))
        xnT = sb(pa, "xnT", [128, 8, NSEQ * XW], BF16)
        b_xnT = bufs("xnT", NT)
        b_pad = Buf("xnTpad")
        NXB = 6
        xt = [sb(pa, f"xt{i}", [128, D], F32) for i in range(NXB)]
        b_xt = bufs("xt", NXB)
        junk = sb(pa, "junkA", [128, D], BF16)
        b_junk = Buf("junk")
        xnb = [sb(pa, f"xnb{i}", [128, D], BF16) for i in range(2)]
        b_xnb = bufs("xnb", 2)
        ss = sb(pa, "ssA", [128, NT], F32)
        rt = sb(pa, "rtA", [128, NT], F32)
        rstd = sb(pa, "rstdA", [128, NT], F32)
        b_ss = bufs("ss", NT // 4)
        b_rt = bufs("rt", NT // 4)
        b_rstd = bufs("rstd", NT // 4)
        pT = [ps(pa, f"pT{i}", [128, 8, 128], BF16) for i in range(2)]
        b_pT = bufs("pT", 2)
        acc = [ps(pa, f"accA{i}", [128, 512], F32) for i in range(4)]
        b_acc = bufs("accA", 4)

        gmix = sb(pa, "gmix", [128, 8], F32)
        cwb = sb(pa, "cwb", [128, 3, HYC], F32)
        cbb = sb(pa, "cbb", [128, HYC], F32)
        b_small = Buf("smallA")
        S.dma("sp", lambda e: e.dma_start(out=gmix[:], in_=gmix_pc[:, :]), writes=[b_small])
        S.dma("sp", lambda e: e.dma_start(
            out=cwb[:].rearrange("p a n -> p (a n)"),
            in_=conv_w.rearrange("a n -> (a n)").partition_broadcast(128)), writes=[b_small])
        S.dma("sp", lambda e: e.dma_start(
            out=cbb[:], in_=conv_b.rearrange("a n -> (a n)").partition_broadcast(128)), writes=[b_small])

        def _pads(e):
            ins = None
            for b in range(NSEQ):
                ins = e.memset(xnT[:, :, b * XW:b * XW + 1], 0.0)
                ins = e.memset(xnT[:, :, b * XW + XW - 1:b * XW + XW], 0.0)
            return ins
        S.op("pool", _pads, writes=[b_pad])

        wst = [sb(pa, f"wst{i}", [128, 8, 512], F32) for i in range(2)]
        b_wst = bufs("wst", 2)
        wg = [[sb(pa, f"wg{i}_{s}", [128, 8, 512], BF16) for s in range(3)] for i in range(2)]
        b_wg = bufs("wg", 2)
        ut = [sb(pa, f"ut{i}", [128, 512], F32) for i in range(2)]
        b_ut = bufs("ut", 2)
        vt = [sb(pa, f"vt{i}", [128, NAH, 65], BF16) for i in range(2)]
        b_vt = bufs("vt", 2)
        qt = [sb(pa, f"qt{i}", [128, 512], BF16) for i in range(2)]
        b_qt = bufs("qt", 2)
        for i in range(2):
            S.op("pool", lambda e, i=i: e.memset(vt[i][:], 1.0), writes=[b_vt[i]])
        w_in_v = w_in.rearrange("(c p) n -> p c n", p=128)
        b_Uhy = [bufs(f"Uhy{g}_", NT) for g in range(3)]
        b_QT = Buf("QT")
        b_KT = Buf("KT")
        b_VA = bufs("VA", NT)
        na_ = [0]

        def load_group(g):
            gb = g % 2
            S.dma("sp", lambda e: e.dma_start(out=wst[gb][:], in_=w_in_v[:, :, g * 512:(g + 1) * 512]),
                  writes=[b_wst[gb]])

        def prep_group(g):
            gb = g % 2
            if g < 3:
                def _prep(e):
                    ins = None
                    for s in range(3):
                        for c in range(8):
                            ins = e.scalar_tensor_tensor(
                                out=wg[gb][s][:, c, :], in0=wst[gb][:, c, :], scalar=gmix[:, c:c + 1],
                                in1=cwb[:, s, g * 512:(g + 1) * 512], op0=ALU.mult, op1=ALU.mult)
                    return ins
            else:
                def _prep(e):
                    ins = None
                    sc = 0.125 if g == 3 else 1.0
                    for c in range(8):
                        ins = e.tensor_scalar(out=wg[gb][0][:, c, :], in0=wst[gb][:, c, :],
                                              scalar1=gmix[:, c:c + 1], scalar2=sc, op0=ALU.mult, op1=ALU.mult)
                    return ins
            S.op("dve", _prep, reads=[b_wst[gb], b_small], writes=[b_wg[gb]])

        def tile_unit(g, j):
            gb = g % 2
            bq, tq = j // 16, j % 16
            a = na_[0] % 4
            na_[0] += 1
            nsh = 3 if g < 3 else 1

            def _mm(e):
                ins = None
                n = 0
                tot = nsh * 8
                for s in range(nsh):
                    sh = s if nsh == 3 else 1
                    c0 = bq * XW + tq * 128 + sh
                    for c in range(8):
                        ins = e.matmul(acc[a][:], lhsT=xnT[:, c, c0:c0 + 128], rhs=wg[gb][s][:, c, :],
                                       start=(n == 0), stop=(n == tot - 1))
                        n += 1
                return ins
            rd = [b_wg[gb], b_xnT[j], b_pad]
            if j > 0:
                rd.append(b_xnT[j - 1])
            if j < NT - 1:
                rd.append(b_xnT[j + 1])
            S.op("pe", _mm, reads=rd, writes=[b_acc[a]])
            if g < 3:
                ub = j % 2
                S.op("dve", lambda e: e.tensor_tensor(
                    out=ut[ub][:], in0=acc[a][:], in1=cbb[:, g * 512:(g + 1) * 512], op=ALU.add),
                    reads=[b_acc[a], b_small], writes=[b_ut[ub]])
                S.dma("pool", lambda e: e.dma_start(
                    out=U_hy[j * 128:(j + 1) * 128, g * 512:(g + 1) * 512], in_=ut[ub][:]),
                    reads=[b_ut[ub]], writes=[b_Uhy[g][j]])
            else:
                vb = j % 2
                S.op("act", lambda e: e.copy(
                    out=vt[vb][:, :, 0:64], in_=acc[a][:].rearrange("p (h d) -> p h d", h=NAH)),
                    reads=[b_acc[a]], writes=[b_vt[vb]])
                S.dma("pool", lambda e: e.dma_start(
                    out=VA[j * 128:(j + 1) * 128, :], in_=vt[vb][:].rearrange("p h d -> p (h d)")),
                    reads=[b_vt[vb]], writes=[b_VA[j]])

        load_group(0)
        prep_group(0)
        load_group(1)
        done0 = 0
        for g4 in range(NT // 4):
            for q in range(4):
                j = g4 * 4 + q
                xb = j % NXB
                S.dma("sp", lambda e, j=j, xb=xb: e.dma_start(out=xt[xb][:], in_=x[j * 128:(j + 1) * 128, :]),
                      writes=[b_xt[xb]])
                S.op("act", lambda e, j=j, xb=xb: e.activation(
                    out=junk[:], in_=xt[xb][:], func=AF.Square, accum_out=ss[:, j:j + 1]),
                    reads=[b_xt[xb]], writes=[b_junk, b_ss[g4]])
            S.op("act", lambda e, g4=g4: e.activation(
                out=rt[:, g4 * 4:g4 * 4 + 4], in_=ss[:, g4 * 4:g4 * 4 + 4], func=AF.Sqrt,
                scale=1.0 / D, bias=EPS), reads=[b_ss[g4]], writes=[b_rt[g4]])
            S.op("dve", lambda e, g4=g4: e.reciprocal(out=rstd[:, g4 * 4:g4 * 4 + 4], in_=rt[:, g4 * 4:g4 * 4 + 4]),
                 reads=[b_rt[g4]], writes=[b_rstd[g4]])
            for q in range(4):
                j = g4 * 4 + q
                xb = j % NXB
                nb = j % 2
                S.op("dve", lambda e, j=j, xb=xb, nb=nb: e.tensor_scalar(
                    out=xnb[nb][:], in0=xt[xb][:], scalar1=rstd[:, j:j + 1], scalar2=None, op0=ALU.mult),
                    reads=[b_xt[xb], b_rstd[g4]], writes=[b_xnb[nb]])

                def _tr(e, nb=nb):
                    ins = None
                    for c in range(8):
                        ins = e.transpose(out=pT[nb][:, c, :], in_=xnb[nb][:, c * 128:(c + 1) * 128],
                                          identity=ident_bf[:])
                    return ins
                S.op("pe", _tr, reads=[b_xnb[nb], b_ident], writes=[b_pT[nb]])
                bq, tq = j // 16, j % 16
                col = bq * XW + 1 + tq * 128
                S.op("act", lambda e, nb=nb, col=col: e.copy(out=xnT[:, :, col:col + 128], in_=pT[nb][:]),
                     reads=[b_pT[nb]], writes=[b_xnT[j]])
            ready = g4 * 4 - 1 if g4 < NT // 4 - 1 else NT
            while done0 < ready:
                tile_unit(0, done0)
                done0 += 1
            if g4 == 0:
                prep_group(1)
                load_group(2)
        while done0 < NT:
            tile_unit(0, done0)
            done0 += 1

        for g in range(1, 6):
            gb = g % 2
            if g + 1 < 6:
                prep_group(g + 1)
            if g + 2 < 6:
                load_group(g + 2)
            if g < 3 or g == 5:
                for j in range(NT):
                    tile_unit(g, j)
            else:
                dst, b_dst = (QT, b_QT) if g == 3 else (KT, b_KT)
                for fc in range(4):
                    for tg in range(8):
                        bq, t0 = tg // 4, (tg % 4) * 512
                        c0 = bq * XW + 1 + t0
                        a = na_[0] % 4
                        na_[0] += 1

                        def _mm(e, gb=gb, a=a, fc=fc, c0=c0):
                            ins = None
                            for c in range(8):
                                ins = e.matmul(acc[a][:], lhsT=wg[gb][0][:, c, fc * 128:(fc + 1) * 128],
                                               rhs=xnT[:, c, c0:c0 + 512], start=(c == 0), stop=(c == 7))
                            return ins
                        S.op("pe", _mm, reads=[b_wg[gb]] + b_xnT[tg * 4:tg * 4 + 4], writes=[b_acc[a]])
                        qb = na_[0] % 2
                        S.op("act", lambda e, a=a, qb=qb: e.copy(out=qt[qb][:], in_=acc[a][:]),
                             reads=[b_acc[a]], writes=[b_qt[qb]])
                        S.dma("pool", lambda e, dst=dst, fc=fc, tg=tg, qb=qb: e.dma_start(
                            out=dst[fc, :, tg * 512:(tg + 1) * 512], in_=qt[qb][:]),
                            reads=[b_qt[qb]], writes=[b_dst])
        out_bufs += [bb for g in range(3) for bb in b_Uhy[g]] + [b_QT, b_KT] + b_VA
        S.barrier()

    if stop_after == "A":
        S.finish("sp", out_bufs)
        return nc, es

    b_K = [bufs(f"K{o}_", 16) for o in range(2)]
    TWO_PI = 2.0 * math.pi
    with ExitStack() as pf:
        pf.enter_context(nc.named_scope("phF"))
        zT = sb(pf, "zTs", [33, L], F32)
        w1s = sb(pf, "w1s", [33, 64], F32)
        w2s = sb(pf, "w2s", [64, 64], F32)
        w3s = sb(pf, "w3s", [64, 2048], BF16)
        fcol = sb(pf, "fcol_s", [64, 4], F32)
        fb = sb(pf, "fb", [64, 2], F32)
        b_fc = Buf("fconst")
        b_fb = Buf("fb")
        for dst, srcd in ((zT, zT_d), (w1s, fw1_d), (w2s, fw2_d), (fcol, fcol_d)):
            S.dma("sp", lambda e, dst=dst, srcd=srcd: e.dma_start(out=dst[:], in_=srcd[:, :]), writes=[b_fc])
        b_w3s = Buf("w3s")
        S.dma("pool", lambda e: e.dma_start(out=w3s[:], in_=fw3_d[:, :]), writes=[b_w3s])

        def _fb(e):
            e.tensor_tensor(out=fb[:, 0:1], in0=fcol[:, 0:1], in1=fcol[:, 1:2], op=ALU.mult)
            return e.tensor_tensor(out=fb[:, 1:2], in0=fcol[:, 2:3], in1=fcol[:, 3:4], op=ALU.mult)
        S.op("dve", _fb, reads=[b_fc], writes=[b_fb])
        zt = sb(pf, "zerot", [128, 8192], BF16)
        b_zt = Buf("zerot")
        S.op("pool", lambda e: e.memset(zt[:], 0.0), writes=[b_zt])
        b_XS = bufs("XS", NE)
        XSv = XS.rearrange("(p r) d -> p (r d)", p=128)
        for i in range(16):
            S.dma("pool", lambda e, i=i: e.dma_start(out=XSv[:, i * 8192:(i + 1) * 8192], in_=zt[:]),
                  reads=[b_zt], writes=[b_XS[i]])
        b_XSall = b_XS[:16]
        hid = [sb(pf, "hid0", [64, L], F32), sb(pf, "hid1", [64, L], BF16)]
        b_hid = [bufs(f"hid{i}_", 4) for i in range(2)]
        arg = [sb(pf, f"arg{i}", [64, 512], F32) for i in range(2)]
        m1 = [sb(pf, f"m1_{i}", [64, 512], F32) for i in range(2)]
        m2 = [sb(pf, f"m2_{i}", [64, 512], F32) for i in range(2)]
        b_arg = bufs("arg", 2)
        b_m = bufs("mm", 2)
        fps = [ps(pf, f"fps{i}", [128, 512], F32) for i in range(6)]
        b_fps = bufs("fps", 6)
        for layer in range(2):
            for g in range(4):
                ab = g % 2
                if layer == 0:
                    S.op("pe", lambda e, g=g, ab=ab: e.matmul(
                        fps[ab][0:64, :], lhsT=w1s[:, :], rhs=zT[:, g * 512:(g + 1) * 512], start=True, stop=True),
                        reads=[b_fc], writes=[b_fps[ab]])
                else:
                    S.op("pe", lambda e, g=g, ab=ab: e.matmul(
                        fps[ab][0:64, :], lhsT=w2s[:, :], rhs=hid[0][:, g * 512:(g + 1) * 512], start=True, stop=True),
                        reads=[b_fc, b_hid[0][g]], writes=[b_fps[ab]])
                S.op("dve", lambda e, ab=ab, layer=layer: e.tensor_scalar(
                    out=arg[ab][:], in0=fps[ab][0:64, :], scalar1=fcol[:, 2 * layer:2 * layer + 1],
                    scalar2=fb[:, layer:layer + 1], op0=ALU.mult, op1=ALU.add),
                    reads=[b_fps[ab], b_fc, b_fb], writes=[b_arg[ab]])

                def _wrap(e, ab=ab):
                    e.tensor_scalar(out=m1[ab][:], in0=arg[ab][:], scalar1=math.pi, scalar2=TWO_PI,
                                    op0=ALU.is_gt, op1=ALU.mult)
                    return e.tensor_scalar(out=m2[ab][:], in0=arg[ab][:], scalar1=-math.pi, scalar2=TWO_PI,
                                           op0=ALU.is_lt, op1=ALU.mult)
                S.op("dve", _wrap, reads=[b_arg[ab]], writes=[b_m[ab]])
                S.op("dve", lambda e, ab=ab: e.tensor_tensor(out=arg[ab][:], in0=arg[ab][:], in1=m1[ab][:],
                                                             op=ALU.subtract),
                     reads=[b_arg[ab], b_m[ab]], writes=[b_arg[ab]])
                S.op("dve", lambda e, ab=ab: e.tensor_tensor(out=arg[ab][:], in0=arg[ab][:], in1=m2[ab][:],
                                                             op=ALU.add),
                     reads=[b_arg[ab], b_m[ab]], writes=[b_arg[ab]])
                S.op("act", lambda e, ab=ab, layer=layer, g=g: e.activation(
                    out=hid[layer][:, g * 512:(g + 1) * 512], in_=arg[ab][:], func=AF.Sin),
                    reads=[b_arg[ab]], writes=[b_hid[layer][g]])

        dbg("hid0", hid[0][:], [64, L], F32, b_hid[0])
        dbg("hid1", hid[1][:], [64, L], BF16, b_hid[1])
        fsd = sb(pf, "fsd", [128, 16, 2, 2, 512], BF16)
        b_fsd = bufs("fsd", 16)
        dfb = [sb(pf, f"dfb{i}", [128, 512], F32) for i in range(2)]
        dbb = [sb(pf, f"dbb{i}", [128, 512], F32) for i in range(2)]
        b_dec = bufs("dec", 2)
        Ft = [sb(pf, f"Ft{i}", [128, 512], F32) for i in range(2)]
        Bt = [sb(pf, f"Bt{i}", [128, 512], F32) for i in range(2)]
        b_FB = bufs("FB", 2)
        nfb = 0
        for tt in range(16):
            db_ = tt % 2
            S.dma("sp", lambda e, tt=tt, db_=db_: e.dma_start(out=dfb[db_][:], in_=decf_d[tt * 128:(tt + 1) * 128, :]),
                  writes=[b_dec[db_]])
            S.dma("sp", lambda e, tt=tt, db_=db_: e.dma_start(out=dbb[db_][:], in_=decb_d[tt * 128:(tt + 1) * 128, :]),
                  writes=[b_dec[db_]])
            for cg in range(4):
                S.op("pe", lambda e, tt=tt, cg=cg: e.matmul(
                    fps[2 + cg][:], lhsT=hid[1][:, tt * 128:(tt + 1) * 128], rhs=w3s[:, cg * 512:(cg + 1) * 512],
                    start=True, stop=True), reads=[b_hid[1][tt // 4], b_w3s], writes=[b_fps[2 + cg]])
            for o in range(2):
                k = nfb % 2
                nfb += 1

                def _fbm(e, o=o, k=k, db_=db_):
                    e.tensor_tensor(out=Ft[k][:], in0=fps[2 + o][:], in1=dfb[db_][:], op=ALU.mult)
                    return e.tensor_tensor(out=Bt[k][:], in0=fps[4 + o][:], in1=dbb[db_][:], op=ALU.mult)
                S.op("dve", _fbm, reads=[b_fps[2 + o], b_fps[4 + o], b_dec[db_]], writes=[b_FB[k]])

                def _sd(e, o=o, k=k, tt=tt):
                    e.tensor_tensor(out=fsd[:, tt, 0, o, :], in0=Ft[k][:], in1=Bt[k][:], op=ALU.add)
                    return e.tensor_tensor(out=fsd[:, tt, 1, o, :], in0=Ft[k][:], in1=Bt[k][:], op=ALU.subtract)
                S.op("pool", _sd, reads=[b_FB[k]], writes=[b_fsd[tt]])

        dbg("fsd", fsd[:].rearrange("p a b c d -> p (a b c d)"), [128, 16 * 2 * 2 * 512], BF16, b_fsd)
        cft = [sb(pf, f"cftF{i}", [128, 16, 128], BF16) for i in range(2)]
        sft = [sb(pf, f"sftF{i}", [128, 16, 128], BF16) for i in range(2)]
        b_cs = bufs("csF", 2)
        kt = [sb(pf, f"ktF{i}", [128, 2, 512], F32) for i in range(2)]
        b_kt = bufs("ktF", 2)
        nk = 0
        for w in range(16):
            wb = w % 2
            S.dma("sp", lambda e, w=w, wb=wb: e.dma_start(out=cft[wb][:].rearrange("p a b -> p (a b)"), in_=Cf_d[w]),
                  writes=[b_cs[wb]])
            S.dma("sp", lambda e, w=w, wb=wb: e.dma_start(out=sft[wb][:].rearrange("p a b -> p (a b)"), in_=Sf_d[w]),
                  writes=[b_cs[wb]])
            for o in range(2):
                for ab_ in range(2):
                    mat = cft if ab_ == 0 else sft

                    def _kmm(e, o=o, ab_=ab_, mat=mat, wb=wb):
                        ins = None
                        for tt in range(16):
                            ins = e.matmul(fps[2 + o * 2 + ab_][:], lhsT=mat[wb][:, tt, :], rhs=fsd[:, tt, ab_, o, :],
                                           start=(tt == 0), stop=(tt == 15))
                        return ins
                    S.op("pe", _kmm, reads=[b_cs[wb]] + b_fsd, writes=[b_fps[2 + o * 2 + ab_]])
                k = nk % 2
                nk += 1

                def _kev(e, o=o, k=k):
                    e.copy(out=kt[k][:, 0, :], in_=fps[2 + o * 2][:])
                    return e.copy(out=kt[k][:, 1, :], in_=fps[2 + o * 2 + 1][:])
                S.op("act", _kev, reads=[b_fps[2 + o * 2], b_fps[2 + o * 2 + 1]], writes=[b_kt[k]])
                S.dma("pool", lambda e, o=o, w=w, k=k: e.dma_start(
                    out=Kscr[o, w], in_=kt[k][:].rearrange("p a c -> p (a c)")),
                    reads=[b_kt[k]], writes=[b_K[o][w]])
        S.barrier()
    out_bufs += b_K[0] + b_K[1]
    if stop_after == "F":
        S.finish("sp", out_bufs)
        return nc, es

    b_Z1 = bufs("Z1_", NT)
    b_MIXh = bufs("MIXh", NT)
    with ExitStack() as ph:
        ph.enter_context(nc.named_scope("phH"))
        zbf = sb(ph, "zbf", [128, NSEQ, 16, 512], BF16)
        b_zbf = bufs("zbf", NT)
        PQ = sb(ph, "PQ", [128, 16, NSEQ, 2, 512], BF16)
        b_PQ = [bufs(f"PQ{b}_", 16) for b in range(NSEQ)]
        cft = [sb(ph, f"cftH{i}", [128, 16, 128], BF16) for i in range(2)]
        sft = [sb(ph, f"sftH{i}", [128, 16, 128], BF16) for i in range(2)]
        b_cs = bufs("csH", 2)
        ktile = [sb(ph, f"ktH{i}", [128, 2, 512], F32) for i in range(2)]
        b_ktile = bufs("ktH", 2)
        skb = sb(ph, "skb", [128, 2, 512], F32)
        ghy = sb(ph, "ghy", [128, 512], F32)
        b_hc = Buf("hconst")
        S.dma("sp", lambda e: e.dma_start(out=skb[:].rearrange("p a c -> p (a c)"),
                                          in_=skip_d.rearrange("a n -> (a n)").partition_broadcast(128)),
              writes=[b_hc])
        S.dma("sp", lambda e: e.dma_start(out=ghy[:], in_=ghy_d.rearrange("a n -> (a n)").partition_broadcast(128)),
              writes=[b_hc])
        hps = [ps(ph, f"hps{i}", [128, 512], F32) for i in range(8)]
        b_hps = bufs("hps", 8)
        NW = 3
        t1 = [sb(ph, f"t1_{i}", [128, 512], F32) for i in range(NW)]
        t2 = [sb(ph, f"t2_{i}", [128, 512], F32) for i in range(NW)]
        b_t = bufs("t12_", NW)
        gt = [sb(ph, f"gt{i}", [128, 512], F32) for i in range(NW)]
        zp = [sb(ph, f"zp{i}", [128, 512], F32) for i in range(NW)]
        b_gz = bufs("gz", NW)
        zn = [sb(ph, f"zn{i}", [128, 512], F32) for i in range(NW)]
        b_zn = bufs("zn", NW)
        mixt = [sb(ph, f"mixt{i}", [128, 512], BF16) for i in range(2)]
        b_mixt = bufs("mixt", 2)
        junkh = sb(ph, "junkh", [128, 512], BF16)
        b_junkh = Buf("junkh")
        sq = sb(ph, "sqh", [128, NT], F32)
        sr = sb(ph, "srh", [128, NT], F32)
        srr = sb(ph, "srrh", [128, NT], F32)
        b_sq = bufs("sqh", NT)

        S.dma("pool", lambda e: e.dma_start(
            out=zbf[:].rearrange("p b t c -> p (b t) c"),
            in_=U_hy.rearrange("(j p) c -> p j c", p=128)[:, :, 2 * HYW:3 * HYW]),
            reads=b_Uhy[2], writes=b_zbf)

        nt_ = 0
        for o in range(2):
            for w in range(16):
                wb = w % 2
                S.dma("sp", lambda e, w=w, wb=wb: e.dma_start(out=cft[wb][:].rearrange("p a b -> p (a b)"), in_=Cf_d[w]),
                      writes=[b_cs[wb]])
                S.dma("sp", lambda e, w=w, wb=wb: e.dma_start(out=sft[wb][:].rearrange("p a b -> p (a b)"), in_=Sf_d[w]),
                      writes=[b_cs[wb]])
                S.dma("sp", lambda e, o=o, w=w, wb=wb: e.dma_start(out=ktile[wb][:].rearrange("p a c -> p (a c)"),
                                                                 in_=Kscr[o, w]),
                      reads=[b_K[o][w]], writes=[b_ktile[wb]])
                for b in range(NSEQ):
                    pa_, pb_ = wb * 4 + b * 2, wb * 4 + b * 2 + 1
                    for ab_, bank in ((0, pa_), (1, pb_)):
                        mat = cft if ab_ == 0 else sft

                        def _fmm(e, mat=mat, wb=wb, b=b, bank=bank):
                            ins = None
                            for tt in range(16):
                                ins = e.matmul(hps[bank][:], lhsT=mat[wb][:, tt, :], rhs=zbf[:, b, tt, :],
                                               start=(tt == 0), stop=(tt == 15))
                            return ins
                        S.op("pe", _fmm, reads=[b_cs[wb]] + b_zbf[b * 16:(b + 1) * 16], writes=[b_hps[bank]])
                    for pq in range(2):
                        k = nt_ % NW
                        nt_ += 1
                        ka, kb = (0, 1) if pq == 0 else (1, 0)

                        def _pr(e, k=k, pa_=pa_, pb_=pb_, wb=wb, ka=ka, kb=kb):
                            e.tensor_tensor(out=t1[k][:], in0=hps[pa_][:], in1=ktile[wb][:, ka, :], op=ALU.mult)
                            return e.tensor_tensor(out=t2[k][:], in0=hps[pb_][:], in1=ktile[wb][:, kb, :], op=ALU.mult)
                        S.op("dve", _pr, reads=[b_hps[pa_], b_hps[pb_], b_ktile[wb]], writes=[b_t[k]])
                        S.op("pool", lambda e, k=k, w=w, b=b, pq=pq: e.tensor_tensor(
                            out=PQ[:, w, b, pq, :], in0=t1[k][:], in1=t2[k][:],
                            op=(ALU.subtract if pq == 0 else ALU.add)),
                            reads=[b_t[k]], writes=[b_PQ[b][w]])
            for tt in range(16):
                tb = tt % 2
                S.dma("sp", lambda e, tt=tt, tb=tb: e.dma_start(out=cft[tb][:].rearrange("p a b -> p (a b)"), in_=Ci_d[tt]),
                      writes=[b_cs[tb]])
                S.dma("sp", lambda e, tt=tt, tb=tb: e.dma_start(out=sft[tb][:].rearrange("p a b -> p (a b)"), in_=Si_d[tt]),
                      writes=[b_cs[tb]])
                for b in range(NSEQ):
                    j = b * 16 + tt
                    bank = (tt * 2 + b) % 8

                    def _imm(e, tb=tb, b=b, bank=bank):
                        ins = None
                        n = 0
                        for w in range(16):
                            for pq, mat in ((0, cft), (1, sft)):
                                ins = e.matmul(hps[bank][:], lhsT=mat[tb][:, w, :], rhs=PQ[:, w, b, pq, :],
                                               start=(n == 0), stop=(n == 31))
                                n += 1
                        return ins
                    S.op("pe", _imm, reads=[b_cs[tb]] + b_PQ[b], writes=[b_hps[bank]])
                    k = nt_ % NW
                    nt_ += 1
                    S.dma("sp", lambda e, j=j, o=o, k=k: e.dma_start(
                        out=gt[k][:], in_=U_hy[j * 128:(j + 1) * 128, o * HYW:(o + 1) * HYW]),
                        reads=[b_Uhy[o][j]], writes=[b_gz[k]])
                    if o == 0:
                        S.dma("sp", lambda e, j=j, k=k: e.dma_start(
                            out=zp[k][:], in_=U_hy[j * 128:(j + 1) * 128, 2 * HYW:3 * HYW]),
                            reads=[b_Uhy[2][j]], writes=[b_gz[k]])
                    else:
                        S.dma("sp", lambda e, j=j, k=k: e.dma_start(out=zp[k][:], in_=Z1[j * 128:(j + 1) * 128, :]),
                              reads=[b_Z1[j]], writes=[b_gz[k]])
                    S.op("pool", lambda e, k=k, o=o: e.tensor_tensor(out=t1[k][:], in0=zp[k][:], in1=skb[:, o, :],
                                                                     op=ALU.mult),
                         reads=[b_gz[k], b_hc], writes=[b_t[k]])
                    S.op("dve", lambda e, k=k, bank=bank: e.tensor_tensor(out=t2[k][:], in0=hps[bank][:], in1=t1[k][:],
                                                                          op=ALU.add),
                         reads=[b_hps[bank], b_t[k]], writes=[b_t[k]])
                    S.op("dve", lambda e, k=k: e.tensor_tensor(out=zn[k][:], in0=t2[k][:], in1=gt[k][:], op=ALU.mult),
                         reads=[b_t[k], b_gz[k]], writes=[b_zn[k]])
                    if o == 0:
                        S.dma("pool", lambda e, j=j, k=k: e.dma_start(out=Z1[j * 128:(j + 1) * 128, :], in_=zn[k][:]),
                              reads=[b_zn[k]], writes=[b_Z1[j]])
                        S.op("act", lambda e, k=k, b=b, tt=tt: e.copy(out=zbf[:, b, tt, :], in_=zn[k][:]),
                             reads=[b_zn[k]], writes=[b_zbf[j]])
                    else:
                        S.op("act", lambda e, k=k, j=j: e.activation(
                            out=junkh[:], in_=zn[k][:], func=AF.Square, accum_out=sq[:, j:j + 1]),
                            reads=[b_zn[k]], writes=[b_junkh, b_sq[j]])
                        S.op("act", lambda e, j=j: e.activation(
                            out=sr[:, j:j + 1], in_=sq[:, j:j + 1], func=AF.Sqrt, scale=1.0 / HYW, bias=EPS),
                            reads=[b_sq[j]], writes=[b_sq[j]])
                        S.op("dve", lambda e, j=j: e.reciprocal(out=srr[:, j:j + 1], in_=sr[:, j:j + 1]),
                             reads=[b_sq[j]], writes=[b_sq[j]])
                        mb = j % 2
                        S.op("dve", lambda e, k=k, j=j, mb=mb: e.scalar_tensor_tensor(
                            out=mixt[mb][:], in0=zn[k][:], scalar=srr[:, j:j + 1], in1=ghy[:],
                            op0=ALU.mult, op1=ALU.mult),
                            reads=[b_zn[k], b_sq[j], b_hc], writes=[b_mixt[mb]])
                        S.dma("pool", lambda e, j=j, mb=mb: e.dma_start(
                            out=MIX[j * 128:(j + 1) * 128, 0:HYW], in_=mixt[mb][:]),
                            reads=[b_mixt[mb]], writes=[b_MIXh[j]])
        S.barrier()
    out_bufs += b_Z1 + b_MIXh
    if stop_after == "H":
        S.finish("sp", out_bufs)
        return nc, es

    b_MIXn = bufs("MIXn", NT)
    with ExitStack() as pn:
        pn.enter_context(nc.named_scope("phN"))
        nmask = sb(pn, "nmask", [128, 5, 640], F32)
        gna = sb(pn, "gna", [128, 512], F32)
        b_nc = Buf("nconst")
        b_nc2 = Buf("nconst2")
        S.dma("sp", lambda e: e.dma_start(out=nmask[:].rearrange("p a c -> p (a c)"), in_=nmask_d[:, :]), writes=[b_nc])
        S.dma("sp", lambda e: e.dma_start(out=gna[:], in_=gna_d.rearrange("a n -> (a n)").partition_broadcast(128)),
              writes=[b_nc2])
        rpbT = [sb(pn, f"rpbT{i}", [128, 18 * 64], F32) for i in range(2)]
        b_rpbT = bufs("rpbT", 2)
        CB = [sb(pn, f"CB{i}", [128, 5, 640], F32) for i in range(2)]
        b_CB = bufs("CB", 2)
        qTs = [sb(pn, f"qTs{i}", [64, L], BF16) for i in range(2)]
        kTs = [sb(pn, f"kTs{i}", [64, L], BF16) for i in range(2)]
        b_qk = bufs("qk", 2)
        b_qk2 = bufs("qk2_", 2)
        vA = [sb(pn, f"vA{i}", [128, 16, NAH * 65], BF16) for i in range(2)]
        b_vA = bufs("vA", 2)
        ytile = [sb(pn, f"ytile{i}", [128, 16, 512], F32) for i in range(2)]
        b_yt = [bufs(f"yt{i}_", 16) for i in range(2)]
        sps = [ps(pn, f"sps{i}", [128, 1024], F32) for i in range(2)]
        b_sps = bufs("sps", 2)
        ops_ = [ps(pn, f"ops{i}", [128, 512], F32) for i in range(2)]
        b_ops = bufs("ops", 2)
        ssb = [sb(pn, f"ssb{i}", [128, 640], F32) for i in range(2)]
        b_ssb = bufs("ssb", 2)
        pTs = [sb(pn, f"pTs{i}", [128, 640], BF16) for i in range(4)]
        b_pTs = bufs("pTs", 4)
        rc = sb(pn, "rcn", [128, 4], F32)
        b_rc = bufs("rcn", 4)
        junkn = sb(pn, "junkn", [128, 512], BF16)
        b_junkn = Buf("junkn")
        sqn = sb(pn, "sqn", [128, NT], F32)
        srn = sb(pn, "srn", [128, NT], F32)
        srrn = sb(pn, "srrn", [128, NT], F32)
        b_sqn = bufs("sqn", NT)
        mixn = [sb(pn, f"mixn{i}", [128, 512], BF16) for i in range(2)]
        b_mixn = bufs("mixn", 2)
        TYPE_OF = {0: 0, 1: 1, 14: 3, 15: 4}
        OFF_OF = {0: 0, 1: 2, 2: 4, 3: 6, 4: 8}
        for b in range(NSEQ):
            vb = b % 2
            S.dma("sp", lambda e, b=b, vb=vb: e.dma_start(
                out=vA[vb][:], in_=VA.rearrange("(j p) c -> p j c", p=128)[:, b * 16:(b + 1) * 16, :]),
                reads=b_VA[b * 16:(b + 1) * 16], writes=[b_vA[vb]])
            items = [(h, p) for h in range(NAH) for p in range(16)]

            def n_st0(idx, b=b, vb=vb):
                h, p = items[idx]
                it = b * len(items) + idx
                hb = (b * NAH + h) % 2
                if p == 0:
                    S.dma("sp", lambda e: e.dma_start(out=rpbT[hb][:], in_=rpbT_d[h]), writes=[b_rpbT[hb]])
                    S.dma("sp", lambda e: e.dma_start(
                        out=qTs[hb][:], in_=QT[h // 2, (h % 2) * 64:(h % 2) * 64 + 64, b * L:(b + 1) * L]),
                        reads=[b_QT], writes=[b_qk[hb]])
                    S.dma("sp", lambda e: e.dma_start(
                        out=kTs[hb][:], in_=KT[h // 2, (h % 2) * 64:(h % 2) * 64 + 64, b * L:(b + 1) * L]),
                        reads=[b_KT], writes=[b_qk2[hb]])

                    def _cb(e):
                        ins = None
                        for ty_ in range(5):
                            off = OFF_OF[ty_] * 64
                            ins = e.tensor_tensor(out=CB[hb][:, ty_, :], in0=rpbT[hb][:, off:off + 640],
                                                  in1=nmask[:, ty_, :], op=ALU.add)
                        return ins
                    S.op("pool", _cb, reads=[b_rpbT[hb], b_nc], writes=[b_CB[hb]])
                P0 = min(max(p - 2, 0), 11)
                ty = TYPE_OF.get(p, 2)
                sb_i = it % 2
                pt_i = it % 4

                def _smm(e):
                    ins = None
                    for jp in range(5):
                        kt_ = P0 + 4 - jp
                        ins = e.matmul(sps[sb_i][:, jp * 128:(jp + 1) * 128],
                                       lhsT=kTs[hb][:, kt_ * 128:(kt_ + 1) * 128],
                                       rhs=qTs[hb][:, p * 128:(p + 1) * 128], start=True, stop=True)
                    return ins
                S.op("pe", _smm, reads=[b_qk[hb], b_qk2[hb]], writes=[b_sps[sb_i]])
                S.op("dve", lambda e: e.tensor_tensor(
                    out=ssb[sb_i][:], in0=sps[sb_i][:, 0:640], in1=CB[hb][:, ty, :], op=ALU.add),
                    reads=[b_sps[sb_i], b_CB[hb]], writes=[b_ssb[sb_i]])
                S.op("act", lambda e: e.activation(out=pTs[pt_i][:], in_=ssb[sb_i][:], func=AF.Exp),
                     reads=[b_ssb[sb_i]], writes=[b_pTs[pt_i]])

            def n_st1(idx, b=b, vb=vb):
                h, p = items[idx]
                it = b * len(items) + idx
                P0 = min(max(p - 2, 0), 11)
                o_i = it % 2
                pt_i = it % 4
                ri = it % 4

                def _omm(e):
                    ins = None
                    for jp in range(5):
                        kt_ = P0 + 4 - jp
                        ins = e.matmul(ops_[o_i][:, 0:65], lhsT=pTs[pt_i][:, jp * 128:(jp + 1) * 128],
                                       rhs=vA[vb][:, kt_, h * 65:(h + 1) * 65], start=(jp == 0), stop=(jp == 4))
                    return ins
                S.op("pe", _omm, reads=[b_pTs[pt_i], b_vA[vb]], writes=[b_ops[o_i]])
                S.op("dve", lambda e: e.reciprocal(out=rc[:, ri:ri + 1], in_=ops_[o_i][:, 64:65]),
                     reads=[b_ops[o_i]], writes=[b_rc[ri]])
                S.op("act", lambda e: e.activation(
                    out=ytile[vb][:, p, h * 64:(h + 1) * 64], in_=ops_[o_i][:, 0:64], func=AF.Copy,
                    scale=rc[:, ri:ri + 1]),
                    reads=[b_ops[o_i], b_rc[ri]], writes=[b_yt[vb][p]])
            pipeline(len(items), [n_st0, n_st1], [0, 2])
            for p in range(16):
                j = b * 16 + p
                S.op("act", lambda e, vb=vb, p=p, j=j: e.activation(
                    out=junkn[:], in_=ytile[vb][:, p, :], func=AF.Square, accum_out=sqn[:, j:j + 1]),
                    reads=[b_yt[vb][p]], writes=[b_junkn, b_sqn[j]])
                S.op("act", lambda e, j=j: e.activation(
                    out=srn[:, j:j + 1], in_=sqn[:, j:j + 1], func=AF.Sqrt, scale=1.0 / HYW, bias=EPS),
                    reads=[b_sqn[j]], writes=[b_sqn[j]])
                S.op("dve", lambda e, j=j: e.reciprocal(out=srrn[:, j:j + 1], in_=srn[:, j:j + 1]),
                     reads=[b_sqn[j]], writes=[b_sqn[j]])
                mb = j % 2
                S.op("dve", lambda e, vb=vb, p=p, j=j, mb=mb: e.scalar_tensor_tensor(
                    out=mixn[mb][:], in0=ytile[vb][:, p, :], scalar=srrn[:, j:j + 1], in1=gna[:],
                    op0=ALU.mult, op1=ALU.mult),
                    reads=[b_yt[vb][p], b_sqn[j], b_nc2], writes=[b_mixn[mb]])
                S.dma("pool", lambda e, j=j, mb=mb: e.dma_start(
                    out=MIX[j * 128:(j + 1) * 128, HYW:2 * HYW], in_=mixn[mb][:]),
                    reads=[b_mixn[mb]], writes=[b_MIXn[j]])
        S.barrier()
    out_bufs += b_MIXn
    if stop_after == "N":
        S.finish("sp", out_bufs)
        return nc, es

    b_H1 = bufs("H1_", NT)
    b_XSw = bufs("XSw", NT)
    with ExitStack() as po:
        po.enter_context(nc.named_scope("phO"))
        woutb = sb(po, "woutb", [128, 8, D], BF16)
        b_wout = Buf("wout")
        S.dma("pool", lambda e: e.dma_start(out=woutb[:], in_=w_out_d.rearrange("(c p) n -> p c n", p=128)),
              writes=[b_wout])
        gffn = sb(po, "gffn", [128, D], F32)
        wr = sb(po, "wr", [128, 8, 36], F32)
        brb = sb(po, "brb", [128, 36], F32)
        ust = sb(po, "ust", [128, 128], F32)
        onesf = sb(po, "onesf", [128, 128], F32)
        ecap = sb(po, "ecap", [128, NE], F32)
        ecapu = sb(po, "ecapu", [128, NE], F32)
        ocum = sb(po, "ocum", [128, NE], F32)
        b_oc = [Buf(f"oconst{i}") for i in range(7)]
        S.dma("sp", lambda e: e.dma_start(out=gffn[:], in_=gffn_d.rearrange("a n -> (a n)").partition_broadcast(128)),
              writes=[b_oc[0]])
        S.dma("sp", lambda e: e.dma_start(out=wr[:], in_=wr_d[:, :, :]), writes=[b_oc[1]])
        S.dma("sp", lambda e: e.dma_start(out=brb[:], in_=br_d.rearrange("a n -> (a n)").partition_broadcast(128)),
              writes=[b_oc[2]])
        S.dma("sp", lambda e: e.dma_start(out=ust[:], in_=ust_d[:, :]), writes=[b_oc[3]])
        S.dma("sp", lambda e: e.dma_start(out=onesf[:], in_=onesf_d[:, :]), writes=[b_oc[4]])
        S.dma("sp", lambda e: e.dma_start(out=ecap[:], in_=ecap_d.rearrange("a n -> (a n)").partition_broadcast(128)),
              writes=[b_oc[5]])
        S.op("dve", lambda e: e.tensor_scalar(out=ecapu[:], in0=ecap[:], scalar1=float(CAP - 1), scalar2=None,
                                              op0=ALU.add), reads=[b_oc[5]], writes=[b_oc[6]])
        b_ocum = Buf("ocum")
        S.op("pool", lambda e: e.memset(ocum[:], 0.0), writes=[b_ocum])

        NB = 6
        NR = 5
        mx = [sb(po, f"mx{i}", [128, D], BF16) for i in range(2)]
        b_mx = bufs("mx", 2)
        mxT = [sb(po, f"mxT{i}", [128, 8, 128], BF16) for i in range(3)]
        b_mxT = bufs("mxT", 3)
        xo = [sb(po, f"xo{i}", [128, D], F32) for i in range(3)]
        b_xo = bufs("xo", 3)
        h1t = [sb(po, f"h1t{i}", [128, D], F32) for i in range(3)]
        b_h1t = bufs("h1t", 3)
        xg = [sb(po, f"xg{i}", [128, D], F32) for i in range(3)]
        b_xg = bufs("xg", 3)
        xgb = [sb(po, f"xgb{i}", [128, D], BF16) for i in range(NB)]
        b_xgb = bufs("xgb", NB)
        xgT = [sb(po, f"xgT{i}", [128, 8, 128], F32) for i in range(2)]
        b_xgT = bufs("xgT", 2)
        junko = sb(po, "junko", [128, D], BF16)
        b_junko = Buf("junko")
        st = sb(po, "sto", [128, NT, 6], F32)
        b_st = bufs("sto", NT)
        lg = [sb(po, f"lg{i}", [128, 36], F32) for i in range(NR)]
        sm = [sb(po, f"sm{i}", [128, 16], F32) for i in range(NR)]
        ohg = [sb(po, f"ohg{i}", [128, 4, 1], F32) for i in range(NR)]
        ge = [sb(po, f"ge{i}", [128, 4], F32) for i in range(NR)]
        tmp48 = [sb(po, f"tmp48{i}", [128, 4, 8], F32) for i in range(NR)]
        el8 = [sb(po, f"el8{i}", [128, 8], F32) for i in range(NR)]
        m8 = [sb(po, f"m8{i}", [128, 8], F32) for i in range(NR)]
        ohk = [[sb(po, f"ohk{i}_{k}", [128, 1, 8], F32) for k in range(2)] for i in range(NR)]
        Ok = [[sb(po, f"Ok{i}_{k}", [128, 4, 8], F32) for k in range(2)] for i in range(NR)]
        Osum = sb(po, "Osum", [128, NT, NE], BF16)
        ustb = sb(po, "ustb", [128, 128], BF16)
        onesb = sb(po, "onesb", [128, 128], BF16)
        ocumb = [sb(po, f"ocumb{i}", [128, NE], BF16) for i in range(2)]
        b_ocumb = bufs("ocumb", 2)
        S.op("pool", lambda e: e.memset(ocumb[0][:], 0.0), writes=[b_ocumb[0]])
        S.op("act", lambda e: e.copy(out=ustb[:], in_=ust[:]), reads=[b_oc[3]], writes=[b_oc[3]])
        S.op("act", lambda e: e.copy(out=onesb[:], in_=onesf[:]), reads=[b_oc[4]], writes=[b_oc[4]])
        b_Osum = bufs("Osum", NT)
        slot = [sb(po, f"slot{i}", [128, NE], F32) for i in range(NR)]
        tmp32 = [sb(po, f"tmp32{i}", [128, NE], F32) for i in range(NR)]
        dk = [sb(po, f"dk{i}", [128, 4], F32) for i in range(NR)]
        b_rt = bufs("rtile", NR)
        tpb = [ps(po, f"tpb{i}", [128, 8, 128], BF16) for i in range(1)]
        b_tpb = bufs("tpb", 1)
        tpf = [ps(po, f"tpf{i}", [128, 8, 128], F32) for i in range(1)]
        b_tpf = bufs("tpf", 1)
        accO = [ps(po, f"accO{i}", [128, 512], F32) for i in range(2)]
        b_accO = bufs("accO", 2)
        rpsLt = [ps(po, f"rpsL{i}", [128, 512], F32) for i in range(2)]
        rpsRt = ps(po, "rpsR", [128, 512], F32)
        b_rpsL = bufs("rpsL", 2)
        b_rpsR1 = Buf("rpsR")
        fl = lambda a: a.rearrange("p g x -> p (g x)")

        def o_st0(j):
            S.dma("sp", lambda e: e.dma_start(out=mx[j % 2][:], in_=MIX[j * 128:(j + 1) * 128, :]),
                  reads=[b_MIXh[j], b_MIXn[j]], writes=[b_mx[j % 2]])
            S.dma("sp", lambda e: e.dma_start(out=xo[j % 3][:], in_=x[j * 128:(j + 1) * 128, :]),
                  writes=[b_xo[j % 3]])

            def _tr(e):
                ins = None
                for c in range(8):
                    ins = e.transpose(out=tpb[0][:, c, :], in_=mx[j % 2][:, c * 128:(c + 1) * 128],
                                      identity=ident_bf[:])
                return ins
            S.op("pe", _tr, reads=[b_mx[j % 2], b_ident], writes=[b_tpb[0]])
            S.op("act", lambda e: e.copy(out=mxT[j % 3][:], in_=tpb[0][:]), reads=[b_tpb[0]], writes=[b_mxT[j % 3]])

        def o_st1(j):
            i3 = j % 3
            for half in range(2):
                a = half

                def _mm(e, half=half, a=a):
                    ins = None
                    for c in range(8):
                        ins = e.matmul(accO[a][:], lhsT=mxT[i3][:, c, :], rhs=woutb[:, c, half * 512:(half + 1) * 512],
                                       start=(c == 0), stop=(c == 7))
                    return ins
                S.op("pe", _mm, reads=[b_mxT[i3], b_wout], writes=[b_accO[a]])
                S.op("dve", lambda e, half=half, a=a: e.tensor_tensor(
                    out=h1t[i3][:, half * 512:(half + 1) * 512], in0=accO[a][:],
                    in1=xo[i3][:, half * 512:(half + 1) * 512], op=ALU.add),
                    reads=[b_accO[a], b_xo[i3]], writes=[b_h1t[i3]])
            S.dma("pool", lambda e: e.dma_start(out=H1[j * 128:(j + 1) * 128, :], in_=h1t[i3][:]),
                  reads=[b_h1t[i3]], writes=[b_H1[j]])
            S.op("act", lambda e: e.activation(out=junko[:], in_=h1t[i3][:], func=AF.Square,
                                               accum_out=st[:, j, 0:1]),
                 reads=[b_h1t[i3]], writes=[b_junko, b_st[j]])

        def o_st1b(j):
            i3 = j % 3
            rsqrt_dve(st[:, j, 0:6], 1.0 / D, [b_st[j]])
            S.op("dve", lambda e: e.scalar_tensor_tensor(
                out=xg[i3][:], in0=h1t[i3][:], scalar=st[:, j, 2:3], in1=gffn[:], op0=ALU.mult, op1=ALU.mult),
                reads=[b_h1t[i3], b_st[j], b_oc[0]], writes=[b_xg[i3]])
            S.op("act", lambda e: e.copy(out=xgb[j % NB][:], in_=xg[i3][:]), reads=[b_xg[i3]], writes=[b_xgb[j % NB]])

        def o_st2(j):
            i3 = j % 3
            i2 = j % 2
            q4 = j % 4

            def _trf(e):
                ins = None
                for c in range(8):
                    ins = e.transpose(out=tpf[0][:, c, :], in_=xg[i3][:, c * 128:(c + 1) * 128], identity=ident_f[:])
                return ins
            S.op("pe", _trf, reads=[b_xg[i3], b_ident], writes=[b_tpf[0]])
            S.op("act", lambda e: e.copy(out=xgT[i2][:], in_=tpf[0][:]), reads=[b_tpf[0]], writes=[b_xgT[i2]])

            def _rmm(e):
                ins = None
                for c in range(8):
                    ins = e.matmul(rpsLt[j % 2][:, 0:36], lhsT=xgT[i2][:, c, :], rhs=wr[:, c, :],
                                   start=(c == 0), stop=(c == 7))
                return ins
            S.op("pe", _rmm, reads=[b_xgT[i2], b_oc[1]], writes=[b_rpsL[j % 2]])

        def o_st3(j):
            i = j % NR
            q4 = j % 4
            S.chain("dve", [
                lambda e: e.tensor_tensor(out=lg[i][:], in0=rpsLt[j % 2][:, 0:36], in1=brb[:], op=ALU.add),
                lambda e: e.tensor_reduce(out=sm[i][:, 0:1], in_=lg[i][:, 0:4], axis=AX.X, op=ALU.max),
                lambda e: e.tensor_scalar(out=sm[i][:, 1:2], in0=sm[i][:, 0:1], scalar1=-1.0, scalar2=None,
                                          op0=ALU.mult),
                lambda e: e.tensor_scalar(out=ohg[i][:, :, 0], in0=lg[i][:, 0:4], scalar1=sm[i][:, 0:1],
                                          scalar2=None, op0=ALU.is_equal),
            ], reads=[b_rpsL[j % 2], b_oc[2]], writes=[b_rt[i]])
            S.op("act", lambda e: e.activation(out=ge[i][:], in_=lg[i][:, 0:4], func=AF.Exp,
                                               bias=sm[i][:, 1:2], accum_out=sm[i][:, 2:3]),
                 reads=[b_rt[i]], writes=[b_rt[i]])

        def o_st3b(j):
            i = j % NR
            S.chain("dve", [
                lambda e: e.reciprocal(out=sm[i][:, 3:4], in_=sm[i][:, 2:3]),
                lambda e: e.tensor_tensor(out=tmp48[i][:], in0=lg[i][:, 4:36].rearrange("p (g x) -> p g x", g=4),
                                          in1=ohg[i][:].broadcast_to([128, 4, 8]), op=ALU.mult),
                lambda e: e.tensor_reduce(out=el8[i][:], in_=tmp48[i][:].rearrange("p g x -> p x g"),
                                          axis=AX.X, op=ALU.add),
                lambda e: e.max(out=m8[i][:], in_=el8[i][:]),
                lambda e: e.tensor_scalar(out=ohk[i][0][:, 0, :], in0=el8[i][:], scalar1=m8[i][:, 0:1],
                                          scalar2=None, op0=ALU.is_equal),
                lambda e: e.tensor_scalar(out=ohk[i][1][:, 0, :], in0=el8[i][:], scalar1=m8[i][:, 1:2],
                                          scalar2=None, op0=ALU.is_equal),
                lambda e: e.tensor_scalar(out=sm[i][:, 4:5], in0=m8[i][:, 0:1], scalar1=-1.0, scalar2=None,
                                          op0=ALU.mult),
            ], reads=[b_rt[i]], writes=[b_rt[i]])
            S.op("act", lambda e: e.activation(out=sm[i][:, 5:6], in_=m8[i][:, 1:2], func=AF.Exp,
                                               bias=sm[i][:, 4:5]),
                 reads=[b_rt[i]], writes=[b_rt[i]])

        def o_st3c(j):
            i = j % NR
            S.chain("dve", [
                lambda e: e.tensor_scalar(out=sm[i][:, 6:7], in0=sm[i][:, 5:6], scalar1=1.0, scalar2=None,
                                          op0=ALU.add),
                lambda e: e.reciprocal(out=sm[i][:, 7:8], in_=sm[i][:, 6:7]),
                lambda e: e.tensor_tensor(out=gates[:, j, 0:1], in0=sm[i][:, 7:8], in1=sm[i][:, 3:4], op=ALU.mult),
                lambda e: e.tensor_tensor(out=gates[:, j, 1:2], in0=gates[:, j, 0:1], in1=sm[i][:, 5:6],
                                          op=ALU.mult),
                lambda e: e.tensor_tensor(out=Ok[i][0][:], in0=ohg[i][:].broadcast_to([128, 4, 8]),
                                          in1=ohk[i][0][:].broadcast_to([128, 4, 8]), op=ALU.mult),
                lambda e: e.tensor_tensor(out=Ok[i][1][:], in0=ohg[i][:].broadcast_to([128, 4, 8]),
                                          in1=ohk[i][1][:].broadcast_to([128, 4, 8]), op=ALU.mult),
                lambda e: e.tensor_tensor(out=Osum[:, j, :], in0=fl(Ok[i][0][:]), in1=fl(Ok[i][1][:]), op=ALU.add),
            ], reads=[b_rt[i]], writes=[b_rt[i], b_route[j], b_Osum[j]])

        def o_st4(j):
            i = j % NR
            q4 = j % 4
            reg = rpsRt[:, 0:NE]

            def _rank(e):
                e.matmul(reg, lhsT=ustb[:], rhs=Osum[:, j, :], start=True, stop=False)
                return e.matmul(reg, lhsT=onesb[:], rhs=ocumb[j % 2][:], start=False, stop=True)
            S.op("pe", _rank, reads=[b_Osum[j], b_ocumb[j % 2], b_oc[3], b_oc[4]], writes=[b_rpsR1])
            S.op("pool", lambda e: e.tensor_tensor(out=ocumb[(j + 1) % 2][:], in0=ocumb[j % 2][:], in1=Osum[:, j, :],
                                                   op=ALU.add),
                 reads=[b_ocumb[j % 2], b_Osum[j]], writes=[b_ocumb[(j + 1) % 2]])
            dfns = [lambda e: e.tensor_tensor(out=slot[i][:], in0=reg, in1=ecap[:], op=ALU.add)]
            for k in range(2):
                dfns += [
                    lambda e, k=k: e.tensor_tensor(out=tmp32[i][:], in0=slot[i][:], in1=fl(Ok[i][k][:]), op=ALU.mult),
                    lambda e, k=k: e.tensor_reduce(out=dk[i][:, k:k + 1], in_=tmp32[i][:], axis=AX.X, op=ALU.add),
                    lambda e, k=k: e.tensor_tensor(out=tmp32[i][:], in0=ecapu[:], in1=fl(Ok[i][k][:]), op=ALU.mult),
                    lambda e, k=k: e.tensor_reduce(out=dk[i][:, 2 + k:3 + k], in_=tmp32[i][:], axis=AX.X, op=ALU.add),
                ]
            dfns += [
                lambda e: e.tensor_tensor(out=dk[i][:, 0:2], in0=dk[i][:, 0:2], in1=dk[i][:, 2:4], op=ALU.min),
                lambda e: e.tensor_copy(out=desti[:, j, :], in_=dk[i][:, 0:2]),
            ]
            S.chain("dve", dfns, reads=[b_rpsR1, b_rt[i], b_oc[5], b_oc[6]], writes=[b_rt[i], b_route[j]])
            for k in range(2):
                S.dma("pool", lambda e, k=k: e.indirect_dma_start(
                    out=XS[:, :], out_offset=bass.IndirectOffsetOnAxis(ap=desti[:, j, k:k + 1], axis=0),
                    in_=xgb[j % NB][:], in_offset=None), reads=[b_xgb[j % NB], b_route[j]] + b_XSall,
                    writes=[b_XSw[j]])
        pipeline(NT, [o_st0, o_st1, o_st1b, o_st2, o_st3, o_st3b, o_st3c, o_st4], [0, 1, 2, 3, 4, 5, 6, 7])
        dbg("desti", desti[:].rearrange("p t k -> p (t k)"), [128, NT * 2], I32, b_route)
        dbg("gates", gates[:].rearrange("p t k -> p (t k)"), [128, NT * 2], F32, b_route)
        S.barrier()
    out_bufs += b_H1 + b_XSw
    if stop_after == "O":
        S.finish("sp", out_bufs)
        return nc, es

    b_YS = bufs("YS", NE * 4)
    with ExitStack() as pe_:
        pe_.enter_context(nc.named_scope("phE"))
        w1b = [sb(pe_, f"w1b{i}", [128, 8, DEXP], BF16) for i in range(2)]
        w3b = [sb(pe_, f"w3b{i}", [128, 8, DEXP], BF16) for i in range(2)]
        w2b = [sb(pe_, f"w2b{i}", [128, 4, D], BF16) for i in range(3)]
        b_w1 = bufs("w1b", 2)
        b_w3 = bufs("w3b", 2)
        b_w2 = bufs("w2b", 3)
        NXS = 6
        xs = [sb(pe_, f"xs{i}", [128, D], BF16) for i in range(NXS)]
        b_xs = bufs("xs", NXS)
        xT = [sb(pe_, f"xTe{i}", [128, 8, CAP], BF16) for i in range(2)]
        b_xT = [bufs(f"xTe{i}_", 4) for i in range(2)]
        s1 = [sb(pe_, f"s1_{i}", [128, CAP], F32) for i in range(2)]
        b_s1 = bufs("s1_", 2)
        hT = [sb(pe_, f"hT{i}", [128, 4, CAP], BF16) for i in range(2)]
        b_hT = [bufs(f"hT{i}_", 4) for i in range(2)]
        yb = [sb(pe_, f"yb{i}", [128, D], F32) for i in range(4)]
        b_yb = bufs("yb", 4)
        b_ybh = [bufs(f"ybh{i}_", 2) for i in range(4)]
        tpe = [ps(pe_, f"tpe{i}", [128, 8, 128], BF16) for i in range(2)]
        b_tpe = bufs("tpe", 2)
        hps_ = [ps(pe_, f"hpsE{i}", [128, 512], F32) for i in range(4)]
        b_hpsE = bufs("hpsE", 4)
        yps = [ps(pe_, f"ypsE{i}", [128, 512], F32) for i in range(2)]
        b_yps = bufs("ypsE", 2)

        def e_st0(ex, blk):
            wb = ex % 2
            if blk == 0:
                S.dma("pool", lambda e: e.dma_start(
                    out=w1b[wb][:], in_=ew1_d[ex].rearrange("(c p) f -> p c f", p=128)), writes=[b_w1[wb]])
                S.dma("pool", lambda e: e.dma_start(
                    out=w3b[wb][:], in_=ew3_d[ex].rearrange("(c p) f -> p c f", p=128)), writes=[b_w3[wb]])
                S.dma("pool", lambda e: e.dma_start(
                    out=w2b[ex % 3][:], in_=ew2_d[ex].rearrange("(c p) f -> p c f", p=128)), writes=[b_w2[ex % 3]])
                for bb in range(4):
                    n_ = ex * 4 + bb
                    S.dma("sp", lambda e, n_=n_: e.dma_start(out=xs[n_ % NXS][:], in_=XS[n_ * 128:n_ * 128 + 128, :]),
                          reads=b_XSw + b_XSall, writes=[b_xs[n_ % NXS]])
            n = ex * 4 + blk
            xi = n % NXS
            ti = n % 2

            def _tr(e):
                ins = None
                for c in range(8):
                    ins = e.transpose(out=tpe[ti][:, c, :], in_=xs[xi][:, c * 128:(c + 1) * 128],
                                      identity=ident_bf[:])
                return ins
            S.op("pe", _tr, reads=[b_xs[xi], b_ident], writes=[b_tpe[ti]])
            S.op("act", lambda e: e.copy(out=xT[wb][:, :, blk * 128:(blk + 1) * 128], in_=tpe[ti][:]),
                 reads=[b_tpe[ti]], writes=[b_xT[wb][blk]])

        def e_st1(ex, fc):
            wb = ex % 2
            pi = (ex * 4 + fc) % 2
            for which, wt, bw in ((0, w1b, b_w1), (1, w3b, b_w3)):
                def _mm(e, wt=wt, bank=pi * 2 + which):
                    ins = None
                    for c in range(8):
                        ins = e.matmul(hps_[bank][:], lhsT=wt[wb][:, c, fc * 128:(fc + 1) * 128],
                                       rhs=xT[wb][:, c, :], start=(c == 0), stop=(c == 7))
                    return ins
                S.op("pe", _mm, reads=[bw[wb]] + b_xT[wb], writes=[b_hpsE[pi * 2 + which]])
            S.op("act", lambda e: e.activation(out=s1[pi][:], in_=hps_[pi * 2][:], func=AF.Silu),
                 reads=[b_hpsE[pi * 2]], writes=[b_s1[pi]])
            S.op("dve", lambda e: e.tensor_tensor(
                out=hT[wb][:, fc, :], in0=hps_[pi * 2 + 1][:], in1=s1[pi][:], op=ALU.mult),
                reads=[b_hpsE[pi * 2 + 1], b_s1[pi]], writes=[b_hT[wb][fc]])

        def e_st2(ex, blk):
            wb = ex % 2
            n = ex * 4 + blk
            yi = n % 4
            for half in range(2):
                def _ymm(e, half=half):
                    ins = None
                    for fc in range(4):
                        ins = e.matmul(yps[half][:], lhsT=hT[wb][:, fc, blk * 128:(blk + 1) * 128],
                                       rhs=w2b[ex % 3][:, fc, half * 512:(half + 1) * 512],
                                       start=(fc == 0), stop=(fc == 3))
                    return ins
                S.op("pe", _ymm, reads=b_hT[wb] + [b_w2[ex % 3]], writes=[b_yps[half]])
                S.op("dve", lambda e, half=half: e.tensor_copy(out=yb[yi][:, half * 512:(half + 1) * 512],
                                                               in_=yps[half][:]),
                     reads=[b_yps[half]], writes=[b_ybh[yi][half]])
            S.dma("pool", lambda e: e.dma_start(out=YS[n * 128:n * 128 + 128, :], in_=yb[yi][:]),
                  reads=b_ybh[yi], writes=[b_YS[n]])
        for s_ in range(NE + 2):
            for q in range(4):
                if 0 <= s_ - 1 < NE:
                    e_st1(s_ - 1, q)
                if 0 <= s_ - 2 < NE:
                    e_st2(s_ - 2, q)
                if s_ < NE:
                    e_st0(s_, q)
        S.barrier()
    out_bufs += b_YS
    if stop_after == "E":
        S.finish("sp", out_bufs)
        return nc, es

    b_out = bufs("out", NT)
    with ExitStack() as pc:
        pc.enter_context(nc.named_scope("phC"))
        wgb = sb(pc, "wgb", [128, 8, D], BF16)
        wpb = sb(pc, "wpb", [128, 2, D], BF16)
        gple = sb(pc, "gple", [128, 8], F32)
        gfin = sb(pc, "gfin", [128, D], F32)
        b_cc = [Buf(f"cconst{i}") for i in range(4)]
        wgst = [sb(pc, f"wgst{i}", [128, 4, D], F32) for i in range(2)]
        b_wgst = bufs("wgst", 2)
        S.dma("sp", lambda e: e.dma_start(out=gple[:], in_=gple_d[:, :]), writes=[b_cc[0]])
        S.dma("sp", lambda e: e.dma_start(out=gfin[:], in_=gfin_d.rearrange("a n -> (a n)").partition_broadcast(128)),
              writes=[b_cc[1]])
        S.dma("pool", lambda e: e.dma_start(out=wpb[:], in_=wproj_d.rearrange("(c p) n -> p c n", p=128)),
              writes=[b_cc[2]])
        wgv = wgate_d.rearrange("(c p) n -> p c n", p=128)
        for hh in range(2):
            S.dma("sp", lambda e, hh=hh: e.dma_start(out=wgst[hh][:], in_=wgv[:, hh * 4:(hh + 1) * 4, :]),
                  writes=[b_wgst[hh]])

            def _wg(e, hh=hh):
                ins = None
                for c4 in range(4):
                    c = hh * 4 + c4
                    ins = e.tensor_scalar(out=wgb[:, c, :], in0=wgst[hh][:, c4, :], scalar1=gple[:, c:c + 1],
                                          scalar2=None, op0=ALU.mult)
                return ins
            S.op("dve", _wg, reads=[b_wgst[hh], b_cc[0]], writes=[b_cc[3]])
        h1c = [sb(pc, f"h1c{i}", [128, D], F32) for i in range(4)]
        y0 = [sb(pc, f"y0_{i}", [128, D], F32) for i in range(4)]
        y1 = [sb(pc, f"y1_{i}", [128, D], F32) for i in range(4)]
        b_h1c = bufs("h1c", 4)
        b_y0 = bufs("y0_", 4)
        b_y1 = bufs("y1_", 4)
        NH2 = 5
        h2 = [sb(pc, f"h2_{i}", [128, D], F32) for i in range(NH2)]
        b_h2 = bufs("h2_", NH2)
        xn3 = [sb(pc, f"xn3_{i}", [128, D], BF16) for i in range(2)]
        b_xn3 = bufs("xn3_", 2)
        xn3T = [sb(pc, f"xn3T{i}", [128, 8, 128], BF16) for i in range(2)]
        b_xn3T = bufs("xn3T", 2)
        pt32 = [sb(pc, f"pt32_{i}", [128, PLE], F32) for i in range(4)]
        ptb = [sb(pc, f"ptb{i}", [128, PLE], BF16) for i in range(2)]
        pTT = [sb(pc, f"pTT{i}", [128, 2, 128], BF16) for i in range(2)]
        b_pt32 = bufs("pt32", 4)
        b_ptb = bufs("ptb", 2)
        b_pTT = bufs("pTT", 2)
        gsb = [sb(pc, f"gsb{i}", [128, D], F32) for i in range(2)]
        b_gsb = bufs("gsb", 2)
        tq = [sb(pc, f"tq{i}", [128, D], F32) for i in range(3)]
        b_tq = bufs("tq", 3)
        ot = [sb(pc, f"ot{i}", [128, D], F32) for i in range(2)]
        b_ot = bufs("ot", 2)
        junkc = sb(pc, "junkc", [128, D], BF16)
        b_junkc = Buf("junkc")
        stc = sb(pc, "stc", [128, NT, 12], F32)
        b_stc = bufs("stc", NT)
        b_stc2 = bufs("stc2_", NT)
        tpc = ps(pc, "tpc", [128, 8, 128], BF16)
        b_tpc = Buf("tpc")
        tpp = ps(pc, "tpp", [128, 8, 128], BF16)
        b_tpp = Buf("tpp")
        gps = [ps(pc, f"gps{i}", [128, 512], F32) for i in range(2)]
        b_gps = bufs("gps", 2)
        pps = [ps(pc, f"pps{i}", [128, 512], F32) for i in range(2)]
        b_pps = bufs("pps", 2)

        def c_m0(j):
            i4 = j % 4
            S.dma("sp", lambda e: e.dma_start(out=h1c[i4][:], in_=H1[j * 128:(j + 1) * 128, :]),
                  reads=[b_H1[j]], writes=[b_h1c[i4]])
            S.dma("sp", lambda e: e.dma_start(out=pt32[i4][:], in_=p_d[j * 128:(j + 1) * 128, :]),
                  writes=[b_pt32[i4]])
            for k, yy, byy in ((0, y0, b_y0), (1, y1, b_y1)):
                S.dma("pool", lambda e, k=k, yy=yy: e.indirect_dma_start(
                    out=yy[i4][:], out_offset=None, in_=YS[:, :],
                    in_offset=bass.IndirectOffsetOnAxis(ap=desti[:, j, k:k + 1], axis=0)),
                    reads=b_YS + [b_route[j]], writes=[byy[i4]])

        def c_m1(j):
            i4, i5 = j % 4, j % NH2
            S.op("dve", lambda e: e.scalar_tensor_tensor(
                out=h2[i5][:], in0=y0[i4][:], scalar=gates[:, j, 0:1], in1=h1c[i4][:], op0=ALU.mult, op1=ALU.add),
                reads=[b_y0[i4], b_h1c[i4], b_route[j]], writes=[b_h2[i5]])
            S.op("dve", lambda e: e.scalar_tensor_tensor(
                out=h2[i5][:], in0=y1[i4][:], scalar=gates[:, j, 1:2], in1=h2[i5][:], op0=ALU.mult, op1=ALU.add),
                reads=[b_y1[i4], b_h2[i5], b_route[j]], writes=[b_h2[i5]])
            S.op("act", lambda e: e.activation(out=junkc[:], in_=h2[i5][:], func=AF.Square,
                                               accum_out=stc[:, j, 0:1]),
                 reads=[b_h2[i5]], writes=[b_junkc, b_stc[j]])

        def c_m2(j):
            i4, i5, i2 = j % 4, j % NH2, j % 2
            rsqrt_dve(stc[:, j, 0:6], 1.0 / D, [b_stc[j]])
            S.op("dve", lambda e: e.tensor_scalar(out=xn3[i2][:], in0=h2[i5][:], scalar1=stc[:, j, 2:3],
                                                  scalar2=None, op0=ALU.mult),
                 reads=[b_h2[i5], b_stc[j]], writes=[b_xn3[i2]])
            S.op("act", lambda e: e.copy(out=ptb[i2][:], in_=pt32[i4][:]), reads=[b_pt32[i4]], writes=[b_ptb[i2]])

        def c_m3(j):
            i2 = j % 2

            def _tr(e):
                ins = None
                for c in range(8):
                    ins = e.transpose(out=tpc[:, c, :], in_=xn3[i2][:, c * 128:(c + 1) * 128], identity=ident_bf[:])
                return ins
            S.op("pe", _tr, reads=[b_xn3[i2], b_ident], writes=[b_tpc])
            S.op("act", lambda e: e.copy(out=xn3T[i2][:], in_=tpc[:]), reads=[b_tpc], writes=[b_xn3T[i2]])

            def _trp(e):
                ins = None
                for c in range(2):
                    ins = e.transpose(out=tpp[:, c, :], in_=ptb[i2][:, c * 128:(c + 1) * 128], identity=ident_bf[:])
                return ins
            S.op("pe", _trp, reads=[b_ptb[i2], b_ident], writes=[b_tpp])
            S.op("act", lambda e: e.copy(out=pTT[i2][:], in_=tpp[:, 0:2, :]), reads=[b_tpp], writes=[b_pTT[i2]])

        def c_m4(j):
            i2, i3 = j % 2, j % 3
            for half in range(2):
                def _gmm(e, half=half):
                    ins = None
                    for c in range(8):
                        ins = e.matmul(gps[half][:], lhsT=xn3T[i2][:, c, :], rhs=wgb[:, c, half * 512:(half + 1) * 512],
                                       start=(c == 0), stop=(c == 7))
                    return ins
                S.op("pe", _gmm, reads=[b_xn3T[i2], b_cc[3]], writes=[b_gps[half]])
                S.op("act", lambda e, half=half: e.activation(
                    out=gsb[i2][:, half * 512:(half + 1) * 512], in_=gps[half][:], func=AF.Sigmoid),
                    reads=[b_gps[half]], writes=[b_gsb[i2]])

                def _pmm(e, half=half):
                    ins = None
                    for c in range(2):
                        ins = e.matmul(pps[half][:], lhsT=pTT[i2][:, c, :], rhs=wpb[:, c, half * 512:(half + 1) * 512],
                                       start=(c == 0), stop=(c == 1))
                    return ins
                S.op("pe", _pmm, reads=[b_pTT[i2], b_cc[2]], writes=[b_pps[half]])
                S.op("dve", lambda e, half=half: e.tensor_tensor(
                    out=tq[i3][:, half * 512:(half + 1) * 512], in0=pps[half][:],
                    in1=gsb[i2][:, half * 512:(half + 1) * 512], op=ALU.mult),
                    reads=[b_pps[half], b_gsb[i2]], writes=[b_tq[i3]])

        def c_m5(j):
            i3, i5 = j % 3, j % NH2
            S.op("pool", lambda e: e.tensor_tensor(out=tq[i3][:], in0=tq[i3][:], in1=h2[i5][:], op=ALU.add),
                 reads=[b_tq[i3], b_h2[i5]], writes=[b_tq[i3]])
            S.op("act", lambda e: e.activation(out=junkc[:], in_=tq[i3][:], func=AF.Square,
                                               accum_out=stc[:, j, 6:7]),
                 reads=[b_tq[i3]], writes=[b_junkc, b_stc2[j]])

        def c_m6(j):
            i3, i2 = j % 3, j % 2
            rsqrt_dve(stc[:, j, 6:12], 1.0 / D, [b_stc2[j]])
            S.op("dve", lambda e: e.scalar_tensor_tensor(
                out=ot[i2][:], in0=tq[i3][:], scalar=stc[:, j, 8:9], in1=gfin[:], op0=ALU.mult, op1=ALU.mult),
                reads=[b_tq[i3], b_stc2[j], b_cc[1]], writes=[b_ot[i2]])
            S.dma("sp", lambda e: e.dma_start(out=out[j * 128:(j + 1) * 128, :], in_=ot[i2][:]),
                  reads=[b_ot[i2]], writes=[b_out[j]])
        pipeline(NT, [c_m0, c_m1, c_m2, c_m3, c_m4, c_m5, c_m6], [0, 2, 3, 4, 5, 6, 7])
        S.barrier()
    out_bufs += b_out

    S.finish("sp", out_bufs)
    return nc, es


def _prep_inputs(inputs):
    cst = _consts()
    x = np.asarray(inputs["x"], dtype=np.float32)
    shared = {
        "w_in": np.ascontiguousarray(inputs["w_in"][0]),
        "gmix_pc": _pc(inputs["g_mix"][0], 8),
        "conv_w": np.ascontiguousarray(inputs["hy_conv_w"][0]),
        "conv_b": np.ascontiguousarray(inputs["hy_conv_b"][0]).reshape(1, HYC),
        "ident_bf": cst["ident_bf"],
        "ident_f": cst["ident_f"],
        "zT": cst["zT"], "dec_f": cst["dec_f"], "dec_b": cst["dec_b"],
        "Cf": cst["Cf"], "Sf": cst["Sf"], "Ci": cst["Ci"], "Si": cst["Si"],
        "fw1": np.ascontiguousarray(inputs["hy_f_w1"][0]),
        "fw2": np.ascontiguousarray(inputs["hy_f_w2"][0]),
        "fw3": np.ascontiguousarray(inputs["hy_f_w3"][0]),
        "fcol": np.ascontiguousarray(np.stack([inputs["hy_f_freq1"][0], inputs["hy_f_b1"][0],
                                               inputs["hy_f_freq2"][0], inputs["hy_f_b2"][0]], axis=1)),
        "skip": np.ascontiguousarray(inputs["hy_skip"][0]).reshape(1, 2 * HYW),
        "ghy": np.ascontiguousarray(inputs["g_out_hy"][0]).reshape(1, HYW),
        "gna": np.ascontiguousarray(inputs["g_out_na"][0]).reshape(1, HYW),
        "w_out": np.ascontiguousarray(inputs["w_out"][0]),
        "gffn": np.ascontiguousarray(inputs["g_ffn"][0]).reshape(1, D),
        "wr_pc": np.ascontiguousarray(np.concatenate([inputs["router_wg"][0], inputs["router_we"][0]], axis=1)
                                      .reshape(8, 128, 36).transpose(1, 0, 2)),
        "br": np.ascontiguousarray(np.concatenate([inputs["router_bg"][0], inputs["router_be"][0]])).reshape(1, 36),
        "ust": np.triu(np.ones((128, 128), np.float32), 1),
        "ones_f": np.ones((128, 128), np.float32),
        "ecap": (np.arange(NE, dtype=np.float32) * CAP).reshape(1, NE),
        "exp_w1": np.ascontiguousarray(inputs["exp_w1"][0]),
        "exp_w3": np.ascontiguousarray(inputs["exp_w3"][0]),
        "exp_w2": np.ascontiguousarray(inputs["exp_w2"][0]),
        "gple_pc": _pc(inputs["g_ple"][0], 8),
        "w_gate": np.ascontiguousarray(inputs["w_ple_gate"][0]),
        "w_proj": np.ascontiguousarray(inputs["w_ple_proj"][0]),
        "gfin": np.ascontiguousarray(inputs["g_final"]).reshape(1, D),
        "nmask": _natten_consts().reshape(128, 5 * 640),
        "rpbT": _rpb_toeplitz(np.asarray(inputs["na_rpb"][0], dtype=np.float32)),
    }
    maps = []
    for c in range(NCORES):
        m = dict(shared)
        m["x"] = np.ascontiguousarray(x[c * NSEQ:(c + 1) * NSEQ].reshape(T, D))
        m["p"] = np.ascontiguousarray(np.asarray(inputs["p"][0, c * NSEQ:(c + 1) * NSEQ], dtype=np.float32).reshape(T, PLE))
        maps.append(m)
    return maps


def kernel(**inputs):
    nc, es = build_nc()
    maps = _prep_inputs(inputs)
    res = run_bass_kernel_spmd(nc, maps, core_ids=list(range(NCORES)))
    es.close()
    outs = [np.asarray(r["out"]).reshape(NSEQ, L, D) for r in res.results]
    return np.concatenate(outs, axis=0).astype(np.float32)
```

```python
import math
from contextlib import ExitStack

import numpy as np
import ml_dtypes

import concourse.bass as bass
import concourse.mybir as mybir
from concourse.bass_utils import run_bass_kernel_spmd

F32 = mybir.dt.float32
BF16 = mybir.dt.bfloat16
I32 = mybir.dt.int32
U32 = mybir.dt.uint32
AF = mybir.ActivationFunctionType
ALU = mybir.AluOpType
AX = mybir.AxisListType

NCORES = 8
D = 1024
L = 2048
NSEQ = 2
T = NSEQ * L
NT = T // 128
HYW = 512
HYC = 1536
NAH = 8
NE = 32
CAP = 512
DEXP = 512
PLE = 256
EPS = 1e-6
XW = L + 2


class Buf:
    __slots__ = ("name", "w", "r")

    def __init__(self, name):
        self.name = name
        self.w = None
        self.r = []


def bufs(prefix, n):
    return [Buf(f"{prefix}{i}") for i in range(n)]


class Sync:
    EPOCH = 8192
    NDSEM = 8

    def __init__(self, nc, es):
        self.nc = nc
        self.es = es
        self.eng = {"pe": nc.tensor, "act": nc.scalar, "dve": nc.vector, "pool": nc.gpsimd, "sp": nc.sync}
        self.cnt = {e: 0 for e in self.eng}
        self.esems = {e: [] for e in self.eng}
        self.seen = {e: {} for e in self.eng}
        self.dsems = {}
        self.dk = {}
        self.nsem = 0

    def _newsem(self, name):
        self.nsem += 1
        return self.es.enter_context(self.nc.semaphore(name))

    def _esem(self, e, epoch):
        lst = self.esems[e]
        while len(lst) <= epoch:
            lst.append(self._newsem(f"s_{e}_{len(lst)}"))
        return lst[epoch]

    def _wait(self, E, tok):
        if tok is None:
            return
        kind = tok[0]
        if kind == "c":
            _, e, c = tok
            if e == "pe" and E == "pe":
                return
            if self.seen[E].get(e, 0) >= c:
                return
            epoch = (c - 1) // self.EPOCH
            val = (c - 1) % self.EPOCH + 1
            self.eng[E].wait_ge(self._esem(e, epoch), val)
            self.seen[E][e] = c
        else:
            _, key, val = tok
            if self.seen[E].get(key, 0) >= val:
                return
            self.eng[E].wait_ge(self.dsems[key], val)
            self.seen[E][key] = val

    def _deps(self, E, reads, writes):
        toks = []
        for b in reads:
            if b.w is not None:
                toks.append(b.w)
        for b in writes:
            if b.w is not None:
                toks.append(b.w)
            toks.extend(b.r)
        best = {}
        for t in toks:
            k = t[1]
            if k not in best or best[k][2] < t[2]:
                best[k] = t
        for t in best.values():
            self._wait(E, t)

    def _mark(self, tok, reads, writes):
        for b in reads:
            if tok[0] == "c":
                b.r = [t for t in b.r if not (t[0] == "c" and t[1] == tok[1])]
            b.r.append(tok)
        for b in writes:
            b.w = tok
            b.r = []

    def op(self, E, fn, reads=(), writes=()):
        self._deps(E, reads, writes)
        ins = fn(self.eng[E])
        self.cnt[E] += 1
        c = self.cnt[E]
        ins.then_inc(self._esem(E, (c - 1) // self.EPOCH), 1)
        tok = ("c", E, c)
        self._mark(tok, reads, writes)
        return tok

    def chain(self, E, fns, reads=(), writes=()):
        link = Buf("chain")
        tok = None
        for fn in fns:
            tok = self.op(E, fn, reads=list(reads) + [link], writes=list(writes) + [link])
        return tok

    def dma(self, Q, fn, reads=(), writes=()):
        k = self.dk.get(Q, 0)
        self.dk[Q] = k + 1
        idx = k % self.NDSEM
        key = (Q, idx)
        if key not in self.dsems:
            self.dsems[key] = self._newsem(f"d_{Q}_{idx}")
        rnd = k // self.NDSEM
        if rnd > 0:
            self._wait(Q, ("d", key, 16 * rnd))
        self._deps(Q, reads, writes)
        ins = fn(self.eng[Q])
        ins.then_inc(self.dsems[key], 16)
        tok = ("d", key, 16 * (rnd + 1))
        self._mark(tok, reads, writes)
        return tok

    def barrier(self):
        for E in self.eng:
            for e in self.eng:
                if e != E and self.cnt[e] > 0:
                    self._wait(E, ("c", e, self.cnt[e]))
            for key in self.dsems:
                q, idx = key
                k = self.dk.get(q, 0)
                n = (k - idx + self.NDSEM - 1) // self.NDSEM
                if n > 0:
                    self._wait(E, ("d", key, 16 * n))

    def finish(self, E, bufs_):
        for b in bufs_:
            self._wait(E, b.w)


def _consts():
    c = {}
    c["ident_bf"] = np.eye(128, dtype=np.float32).astype(ml_dtypes.bfloat16)
    c["ident_f"] = np.eye(128, dtype=np.float32)
    t = np.linspace(0.0, 1.0, L, dtype=np.float32)[:, None]
    w = (2.0 * math.pi * np.arange(L, dtype=np.float32)[:, None] / L).astype(np.float32)
    bands = np.linspace(1e-4, 15.0, 16, dtype=np.float32)[None, :]
    z = np.concatenate([t, np.cos(bands * w), -np.sin(bands * w)], axis=-1).astype(np.float32)
    c["zT"] = np.ascontiguousarray(z.T)
    max_decay = math.log(1e-2) / 0.3
    min_decay = math.log(1e-2) / 1.5
    deltas = np.linspace(min_decay, max_decay, HYW, dtype=np.float32)
    decay = np.exp(-t * np.abs(deltas)[None, :]).astype(np.float32)
    c["dec_f"] = decay
    db = decay.copy()
    db[0, :] = 0.0
    c["dec_b"] = db
    N = 2 * L
    tt = np.arange(L, dtype=np.float64)
    om = (np.arange(L, dtype=np.float64) + 0.5) * (2.0 * math.pi / N)
    ang = np.outer(tt, om)
    Cf = np.cos(ang)
    Sf = np.sin(ang)

    def fwd_layout(M):
        return np.ascontiguousarray(M.reshape(16, 128, 16, 128).transpose(2, 1, 0, 3)).reshape(16, 128, 2048)

    def inv_layout(M):
        return np.ascontiguousarray(M.reshape(16, 128, 16, 128).transpose(0, 3, 2, 1)).reshape(16, 128, 2048)
    bf = ml_dtypes.bfloat16
    c["Cf"] = fwd_layout(Cf).astype(np.float32).astype(bf)
    c["Sf"] = fwd_layout(Sf).astype(np.float32).astype(bf)
    c["Ci"] = inv_layout(Cf * (2.0 / N)).astype(np.float32).astype(bf)
    c["Si"] = inv_layout(Sf * (2.0 / N)).astype(np.float32).astype(bf)
    return c


def _natten_consts():
    rows, GW, WR, WC = 32, 64, 8, 16
    rs = np.clip(np.arange(rows) - WR // 2, 0, rows - WR)
    cs = np.clip(np.arange(GW) - WC // 2, 0, GW - WC)
    masks = {}
    for p in range(16):
        P0 = min(max(p - 2, 0), 11)
        m = np.zeros((128, 5, 128), np.float32)
        ak = np.arange(128) // 64
        kc = np.arange(128) % 64
        a = np.arange(128) // 64
        qc = np.arange(128) % 64
        for jp in range(5):
            kr = 2 * (P0 + 4 - jp) + ak
            qr = 2 * p + a
            rv = (kr[:, None] >= rs[qr][None, :]) & (kr[:, None] < rs[qr][None, :] + WR)
            cv = (kc[:, None] >= cs[qc][None, :]) & (kc[:, None] < cs[qc][None, :] + WC)
            m[:, jp, :] = np.where(rv & cv, 0.0, -30000.0)
        masks[p] = m.reshape(128, 640)
    types = [0, 1, 2, 14, 15]
    for p in range(2, 14):
        assert np.array_equal(masks[p], masks[2])
    return np.ascontiguousarray(np.stack([masks[t] for t in types], axis=1))


def _rpb_toeplitz(rpb):
    H = rpb.shape[0]
    pad = np.zeros((H, 15 + 8, 31 + 128), np.float32)
    pad[:, 4:4 + 15, 64:64 + 31] = rpb
    ak = (np.arange(128) // 64)[:, None, None]
    kc = (np.arange(128) % 64)[:, None, None]
    e = np.arange(18)[None, :, None]
    qc = np.arange(64)[None, None, :]
    dr = 15 + ak - e + np.zeros_like(qc)
    dc = kc - qc + 15 + np.zeros_like(e)
    dr, dc = np.broadcast_arrays(dr, dc)
    out = pad[:, dr + 4, dc + 64]
    return np.ascontiguousarray(out.reshape(H, 128, 18 * 64))


def _pc(v, nchunk):
    return np.ascontiguousarray(np.asarray(v).reshape(nchunk, 128).T)


def build_nc(debug=False, stop_after=None):
    nc = bass.Bass("TRN2", target_bir_lowering=False)
    es = ExitStack()
    S = Sync(nc, es)

    def din(name, shape, dt=F32):
        return nc.dram_tensor(name, list(shape), dt, kind="ExternalInput").ap()

    def dscratch(name, shape, dt=F32):
        kind = "ExternalOutput" if debug else "Internal"
        return nc.dram_tensor(name, list(shape), dt, kind=kind).ap()

    def sb(stack, name, shape, dt):
        return stack.enter_context(nc.sbuf_tensor("sb_" + name, list(shape), dt))

    def ps(stack, name, shape, dt):
        return stack.enter_context(nc.psum_tensor("ps_" + name, list(shape), dt))

    def pipeline(n, stages, skews):
        for s_ in range(n + max(skews)):
            for st_, sk_ in zip(stages, skews):
                jj = s_ - sk_
                if 0 <= jj < n:
                    st_(jj)

    def rsqrt_dve(st6, scale, bl):
        a, y, t_, u_ = st6[:, 1:2], st6[:, 2:3], st6[:, 3:4], st6[:, 4:5]
        fns = [
            lambda e: e.tensor_scalar(out=a, in0=st6[:, 0:1], scalar1=scale, scalar2=EPS, op0=ALU.mult, op1=ALU.add),
            lambda e: e.tensor_scalar(out=y.bitcast(I32), in0=a.bitcast(I32), scalar1=-0.5, scalar2=1597463007.0,
                                      op0=ALU.mult, op1=ALU.add),
        ]
        for _ in range(3):
            fns += [
                lambda e: e.scalar_tensor_tensor(out=t_, in0=y, scalar=a, in1=y, op0=ALU.mult, op1=ALU.mult),
                lambda e: e.tensor_scalar(out=u_, in0=t_, scalar1=-0.5, scalar2=1.5, op0=ALU.mult, op1=ALU.add),
                lambda e: e.tensor_tensor(out=y, in0=y, in1=u_, op=ALU.mult),
            ]
        S.chain("dve", fns, reads=bl, writes=bl)

    def dbg(name, ap, shape, dt, reads):
        if not debug:
            return
        dtn = nc.dram_tensor("dbg_" + name, list(shape), dt, kind="ExternalOutput").ap()
        bb = Buf("dbg_" + name)
        S.dma("sp", lambda e: e.dma_start(out=dtn, in_=ap), reads=reads, writes=[bb])
        out_bufs.append(bb)

    out_bufs = []
    x = din("x", [T, D])
    w_in = din("w_in", [D, 3072])
    gmix_pc = din("gmix_pc", [128, 8])
    conv_w = din("conv_w", [3, HYC])
    conv_b = din("conv_b", [1, HYC])
    ident_bf_d = din("ident_bf", [128, 128], BF16)
    ident_f_d = din("ident_f", [128, 128])
    zT_d = din("zT", [33, L])
    fw1_d = din("fw1", [33, 64])
    fw2_d = din("fw2", [64, 64])
    fw3_d = din("fw3", [64, 2048])
    fcol_d = din("fcol", [64, 4])
    decf_d = din("dec_f", [L, HYW])
    decb_d = din("dec_b", [L, HYW])
    Cf_d = din("Cf", [16, 128, 2048], BF16)
    Sf_d = din("Sf", [16, 128, 2048], BF16)
    Ci_d = din("Ci", [16, 128, 2048], BF16)
    Si_d = din("Si", [16, 128, 2048], BF16)
    skip_d = din("skip", [1, 2 * HYW])
    ghy_d = din("ghy", [1, HYW])
    gna_d = din("gna", [1, HYW])
    w_out_d = din("w_out", [D, D])
    gffn_d = din("gffn", [1, D])
    wr_d = din("wr_pc", [128, 8, 36])
    br_d = din("br", [1, 36])
    ust_d = din("ust", [128, 128])
    onesf_d = din("ones_f", [128, 128])
    ecap_d = din("ecap", [1, NE])
    ew1_d = din("exp_w1", [NE, D, DEXP])
    ew3_d = din("exp_w3", [NE, D, DEXP])
    ew2_d = din("exp_w2", [NE, DEXP, D])
    gple_d = din("gple_pc", [128, 8])
    wgate_d = din("w_gate", [D, D])
    wproj_d = din("w_proj", [PLE, D])
    gfin_d = din("gfin", [1, D])
    p_d = din("p", [T, PLE])
    nmask_d = din("nmask", [128, 5 * 640])
    rpbT_d = din("rpbT", [NAH, 128, 18 * 64])
    out = nc.dram_tensor("out", [T, D], F32, kind="ExternalOutput").ap()

    U_hy = dscratch("U_hy", [T, HYC])
    QT = dscratch("QT", [4, 128, T], BF16)
    KT = dscratch("KT", [4, 128, T], BF16)
    VA = dscratch("VA", [T, NAH * 65], BF16)
    Kscr = dscratch("Kscr", [2, 16, 128, 2 * HYW])
    Z1 = dscratch("Z1", [T, HYW])
    MIX = dscratch("MIX", [T, D], BF16)
    H1 = dscratch("H1", [T, D])
    XS = dscratch("XS", [NE * CAP, D], BF16)
    YS = dscratch("YS", [NE * CAP, D])

    gs = es
    ident_bf = sb(gs, "ident_bf_s", [128, 128], BF16)
    ident_f = sb(gs, "ident_f_s", [128, 128], F32)
    b_ident = Buf("ident")
    S.dma("sp", lambda e: e.dma_start(out=ident_bf[:], in_=ident_bf_d[:, :]), writes=[b_ident])
    S.dma("sp", lambda e: e.dma_start(out=ident_f[:], in_=ident_f_d[:, :]), writes=[b_ident])

    desti = sb(gs, "desti", [128, NT, 2], I32)
    gates = sb(gs, "gates", [128, NT, 2], F32)
    b_route = bufs("route", NT)

    with ExitStack() as pa:
        pa.enter_context(nc.named_scope("phA"))
        xnT = sb(pa, "xnT", [128, 8, NSEQ * XW], BF16)
        b_xnT = bufs("xnT", NT)
        b_pad = Buf("xnTpad")
        NXB = 6
        xt = [sb(pa, f"xt{i}", [128, D], F32) for i in range(NXB)]
        b_xt = bufs("xt", NXB)
        junk = sb(pa, "junkA", [128, D], BF16)
        b_junk = Buf("junk")
        xnb = [sb(pa, f"xnb{i}", [128, D], BF16) for i in range(2)]
        b_xnb = bufs("xnb", 2)
        ss = sb(pa, "ssA", [128, NT], F32)
        rt = sb(pa, "rtA", [128, NT], F32)
        rstd = sb(pa, "rstdA", [128, NT], F32)
        b_ss = bufs("ss", NT // 4)
        b_rt = bufs("rt", NT // 4)
        b_rstd = bufs("rstd", NT // 4)
        pT = [ps(pa, f"pT{i}", [128, 8, 128], BF16) for i in range(2)]
        b_pT = bufs("pT", 2)
        acc = [ps(pa, f"accA{i}", [128, 512], F32) for i in range(4)]
        b_acc = bufs("accA", 4)

        gmix = sb(pa, "gmix", [128, 8], F32)
        cwb = sb(pa, "cwb", [128, 3, HYC], F32)
        cbb = sb(pa, "cbb", [128, HYC], F32)
        b_small = Buf("smallA")
        S.dma("sp", lambda e: e.dma_start(out=gmix[:], in_=gmix_pc[:, :]), writes=[b_small])
        S.dma("sp", lambda e: e.dma_start(
            out=cwb[:].rearrange("p a n -> p (a n)"),
            in_=conv_w.rearrange("a n -> (a n)").partition_broadcast(128)), writes=[b_small])
        S.dma("sp", lambda e: e.dma_start(
            out=cbb[:], in_=conv_b.rearrange("a n -> (a n)").partition_broadcast(128)), writes=[b_small])

        def _pads(e):
            ins = None
            for b in range(NSEQ):
                ins = e.memset(xnT[:, :, b * XW:b * XW + 1], 0.0)
                ins = e.memset(xnT[:, :, b * XW + XW - 1:b * XW + XW], 0.0)
            return ins
        S.op("pool", _pads, writes=[b_pad])

        wst = [sb(pa, f"wst{i}", [128, 8, 512], F32) for i in range(2)]
        b_wst = bufs("wst", 2)
        wg = [[sb(pa, f"wg{i}_{s}", [128, 8, 512], BF16) for s in range(3)] for i in range(2)]
        b_wg = bufs("wg", 2)
        ut = [sb(pa, f"ut{i}", [128, 512], F32) for i in range(2)]
        b_ut = bufs("ut", 2)
        vt = [sb(pa, f"vt{i}", [128, NAH, 65], BF16) for i in range(2)]
        b_vt = bufs("vt", 2)
        qt = [sb(pa, f"qt{i}", [128, 512], BF16) for i in range(2)]
        b_qt = bufs("qt", 2)
        for i in range(2):
            S.op("pool", lambda e, i=i: e.memset(vt[i][:], 1.0), writes=[b_vt[i]])
        w_in_v = w_in.rearrange("(c p) n -> p c n", p=128)
        b_Uhy = [bufs(f"Uhy{g}_", NT) for g in range(3)]
        b_QT = Buf("QT")
        b_KT = Buf("KT")
        b_VA = bufs("VA", NT)
        na_ = [0]

        def load_group(g):
            gb = g % 2
            S.dma("sp", lambda e: e.dma_start(out=wst[gb][:], in_=w_in_v[:, :, g * 512:(g + 1) * 512]),
                  writes=[b_wst[gb]])

        def prep_group(g):
            gb = g % 2
            if g < 3:
                def _prep(e):
                    ins = None
                    for s in range(3):
                        for c in range(8):
                            ins = e.scalar_tensor_tensor(
                                out=wg[gb][s][:, c, :], in0=wst[gb][:, c, :], scalar=gmix[:, c:c + 1],
                                in1=cwb[:, s, g * 512:(g + 1) * 512], op0=ALU.mult, op1=ALU.mult)
                    return ins
            else:
                def _prep(e):
                    ins = None
                    sc = 0.125 if g == 3 else 1.0
                    for c in range(8):
                        ins = e.tensor_scalar(out=wg[gb][0][:, c, :], in0=wst[gb][:, c, :],
                                              scalar1=gmix[:, c:c + 1], scalar2=sc, op0=ALU.mult, op1=ALU.mult)
                    return ins
            S.op("dve", _prep, reads=[b_wst[gb], b_small], writes=[b_wg[gb]])

        def tile_unit(g, j):
            gb = g % 2
            bq, tq = j // 16, j % 16
            a = na_[0] % 4
            na_[0] += 1
            nsh = 3 if g < 3 else 1

            def _mm(e):
                ins = None
                n = 0
                tot = nsh * 8
                for s in range(nsh):
                    sh = s if nsh == 3 else 1
                    c0 = bq * XW + tq * 128 + sh
                    for c in range(8):
                        ins = e.matmul(acc[a][:], lhsT=xnT[:, c, c0:c0 + 128], rhs=wg[gb][s][:, c, :],
                                       start=(n == 0), stop=(n == tot - 1))
                        n += 1
                return ins
            rd = [b_wg[gb], b_xnT[j], b_pad]
            if j > 0:
                rd.append(b_xnT[j - 1])
            if j < NT - 1:
                rd.append(b_xnT[j + 1])
            S.op("pe", _mm, reads=rd, writes=[b_acc[a]])
            if g < 3:
                ub = j % 2
                S.op("dve", lambda e: e.tensor_tensor(
                    out=ut[ub][:], in0=acc[a][:], in1=cbb[:, g * 512:(g + 1) * 512], op=ALU.add),
                    reads=[b_acc[a], b_small], writes=[b_ut[ub]])
                S.dma("pool", lambda e: e.dma_start(
                    out=U_hy[j * 128:(j + 1) * 128, g * 512:(g + 1) * 512], in_=ut[ub][:]),
                    reads=[b_ut[ub]], writes=[b_Uhy[g][j]])
            else:
                vb = j % 2
                S.op("act", lambda e: e.copy(
                    out=vt[vb][:, :, 0:64], in_=acc[a][:].rearrange("p (h d) -> p h d", h=NAH)),
                    reads=[b_acc[a]], writes=[b_vt[vb]])
                S.dma("pool", lambda e: e.dma_start(
                    out=VA[j * 128:(j + 1) * 128, :], in_=vt[vb][:].rearrange("p h d -> p (h d)")),
                    reads=[b_vt[vb]], writes=[b_VA[j]])

        load_group(0)
        prep_group(0)
        load_group(1)
        done0 = 0
        for g4 in range(NT // 4):
            for q in range(4):
                j = g4 * 4 + q
                xb = j % NXB
                S.dma("sp", lambda e, j=j, xb=xb: e.dma_start(out=xt[xb][:], in_=x[j * 128:(j + 1) * 128, :]),
                      writes=[b_xt[xb]])
                S.op("act", lambda e, j=j, xb=xb: e.activation(
                    out=junk[:], in_=xt[xb][:], func=AF.Square, accum_out=ss[:, j:j + 1]),
                    reads=[b_xt[xb]], writes=[b_junk, b_ss[g4]])
            S.op("act", lambda e, g4=g4: e.activation(
                out=rt[:, g4 * 4:g4 * 4 + 4], in_=ss[:, g4 * 4:g4 * 4 + 4], func=AF.Sqrt,
                scale=1.0 / D, bias=EPS), reads=[b_ss[g4]], writes=[b_rt[g4]])
            S.op("dve", lambda e, g4=g4: e.reciprocal(out=rstd[:, g4 * 4:g4 * 4 + 4], in_=rt[:, g4 * 4:g4 * 4 + 4]),
                 reads=[b_rt[g4]], writes=[b_rstd[g4]])
            for q in range(4):
                j = g4 * 4 + q
                xb = j % NXB
                nb = j % 2
                S.op("dve", lambda e, j=j, xb=xb, nb=nb: e.tensor_scalar(
                    out=xnb[nb][:], in0=xt[xb][:], scalar1=rstd[:, j:j + 1], scalar2=None, op0=ALU.mult),
                    reads=[b_xt[xb], b_rstd[g4]], writes=[b_xnb[nb]])

                def _tr(e, nb=nb):
                    ins = None
                    for c in range(8):
                        ins = e.transpose(out=pT[nb][:, c, :], in_=xnb[nb][:, c * 128:(c + 1) * 128],
                                          identity=ident_bf[:])
                    return ins
                S.op("pe", _tr, reads=[b_xnb[nb], b_ident], writes=[b_pT[nb]])
                bq, tq = j // 16, j % 16
                col = bq * XW + 1 + tq * 128
                S.op("act", lambda e, nb=nb, col=col: e.copy(out=xnT[:, :, col:col + 128], in_=pT[nb][:]),
                     reads=[b_pT[nb]], writes=[b_xnT[j]])
            ready = g4 * 4 - 1 if g4 < NT // 4 - 1 else NT
            while done0 < ready:
                tile_unit(0, done0)
                done0 += 1
            if g4 == 0:
                prep_group(1)
                load_group(2)
        while done0 < NT:
            tile_unit(0, done0)
            done0 += 1

        for g in range(1, 6):
            gb = g % 2
            if g + 1 < 6:
                prep_group(g + 1)
            if g + 2 < 6:
                load_group(g + 2)
            if g < 3 or g == 5:
                for j in range(NT):
                    tile_unit(g, j)
            else:
                dst, b_dst = (QT, b_QT) if g == 3 else (KT, b_KT)
                for fc in range(4):
                    for tg in range(8):
                        bq, t0 = tg // 4, (tg % 4) * 512
                        c0 = bq * XW + 1 + t0
                        a = na_[0] % 4
                        na_[0] += 1

                        def _mm(e, gb=gb, a=a, fc=fc, c0=c0):
                            ins = None
                            for c in range(8):
                                ins = e.matmul(acc[a][:], lhsT=wg[gb][0][:, c, fc * 128:(fc + 1) * 128],
                                               rhs=xnT[:, c, c0:c0 + 512], start=(c == 0), stop=(c == 7))
                            return ins
                        S.op("pe", _mm, reads=[b_wg[gb]] + b_xnT[tg * 4:tg * 4 + 4], writes=[b_acc[a]])
                        qb = na_[0] % 2
                        S.op("act", lambda e, a=a, qb=qb: e.copy(out=qt[qb][:], in_=acc[a][:]),
                             reads=[b_acc[a]], writes=[b_qt[qb]])
                        S.dma("pool", lambda e, dst=dst, fc=fc, tg=tg, qb=qb: e.dma_start(
                            out=dst[fc, :, tg * 512:(tg + 1) * 512], in_=qt[qb][:]),
                            reads=[b_qt[qb]], writes=[b_dst])
        out_bufs += [bb for g in range(3) for bb in b_Uhy[g]] + [b_QT, b_KT] + b_VA
        S.barrier()

    if stop_after == "A":
        S.finish("sp", out_bufs)
        return nc, es

    b_K = [bufs(f"K{o}_", 16) for o in range(2)]
    TWO_PI = 2.0 * math.pi
    with ExitStack() as pf:
        pf.enter_context(nc.named_scope("phF"))
        zT = sb(pf, "zTs", [33, L], F32)
        w1s = sb(pf, "w1s", [33, 64], F32)
        w2s = sb(pf, "w2s", [64, 64], F32)
        w3s = sb(pf, "w3s", [64, 2048], BF16)
        fcol = sb(pf, "fcol_s", [64, 4], F32)
        fb = sb(pf, "fb", [64, 2], F32)
        b_fc = Buf("fconst")
        b_fb = Buf("fb")
        for dst, srcd in ((zT, zT_d), (w1s, fw1_d), (w2s, fw2_d), (fcol, fcol_d)):
            S.dma("sp", lambda e, dst=dst, srcd=srcd: e.dma_start(out=dst[:], in_=srcd[:, :]), writes=[b_fc])
        b_w3s = Buf("w3s")
        S.dma("pool", lambda e: e.dma_start(out=w3s[:], in_=fw3_d[:, :]), writes=[b_w3s])

        def _fb(e):
            e.tensor_tensor(out=fb[:, 0:1], in0=fcol[:, 0:1], in1=fcol[:, 1:2], op=ALU.mult)
            return e.tensor_tensor(out=fb[:, 1:2], in0=fcol[:, 2:3], in1=fcol[:, 3:4], op=ALU.mult)
        S.op("dve", _fb, reads=[b_fc], writes=[b_fb])
        zt = sb(pf, "zerot", [128, 8192], BF16)
        b_zt = Buf("zerot")
        S.op("pool", lambda e: e.memset(zt[:], 0.0), writes=[b_zt])
        b_XS = bufs("XS", NE)
        XSv = XS.rearrange("(p r) d -> p (r d)", p=128)
        for i in range(16):
            S.dma("pool", lambda e, i=i: e.dma_start(out=XSv[:, i * 8192:(i + 1) * 8192], in_=zt[:]),
                  reads=[b_zt], writes=[b_XS[i]])
        b_XSall = b_XS[:16]
        hid = [sb(pf, "hid0", [64, L], F32), sb(pf, "hid1", [64, L], BF16)]
        b_hid = [bufs(f"hid{i}_", 4) for i in range(2)]
        arg = [sb(pf, f"arg{i}", [64, 512], F32) for i in range(2)]
        m1 = [sb(pf, f"m1_{i}", [64, 512], F32) for i in range(2)]
        m2 = [sb(pf, f"m2_{i}", [64, 512], F32) for i in range(2)]
        b_arg = bufs("arg", 2)
        b_m = bufs("mm", 2)
        fps = [ps(pf, f"fps{i}", [128, 512], F32) for i in range(6)]
        b_fps = bufs("fps", 6)
        for layer in range(2):
            for g in range(4):
                ab = g % 2
                if layer == 0:
                    S.op("pe", lambda e, g=g, ab=ab: e.matmul(
                        fps[ab][0:64, :], lhsT=w1s[:, :], rhs=zT[:, g * 512:(g + 1) * 512], start=True, stop=True),
                        reads=[b_fc], writes=[b_fps[ab]])
                else:
                    S.op("pe", lambda e, g=g, ab=ab: e.matmul(
                        fps[ab][0:64, :], lhsT=w2s[:, :], rhs=hid[0][:, g * 512:(g + 1) * 512], start=True, stop=True),
                        reads=[b_fc, b_hid[0][g]], writes=[b_fps[ab]])
                S.op("dve", lambda e, ab=ab, layer=layer: e.tensor_scalar(
                    out=arg[ab][:], in0=fps[ab][0:64, :], scalar1=fcol[:, 2 * layer:2 * layer + 1],
                    scalar2=fb[:, layer:layer + 1], op0=ALU.mult, op1=ALU.add),
                    reads=[b_fps[ab], b_fc, b_fb], writes=[b_arg[ab]])

                def _wrap(e, ab=ab):
                    e.tensor_scalar(out=m1[ab][:], in0=arg[ab][:], scalar1=math.pi, scalar2=TWO_PI,
                                    op0=ALU.is_gt, op1=ALU.mult)
                    return e.tensor_scalar(out=m2[ab][:], in0=arg[ab][:], scalar1=-math.pi, scalar2=TWO_PI,
                                           op0=ALU.is_lt, op1=ALU.mult)
                S.op("dve", _wrap, reads=[b_arg[ab]], writes=[b_m[ab]])
                S.op("dve", lambda e, ab=ab: e.tensor_tensor(out=arg[ab][:], in0=arg[ab][:], in1=m1[ab][:],
                                                             op=ALU.subtract),
                     reads=[b_arg[ab], b_m[ab]], writes=[b_arg[ab]])
                S.op("dve", lambda e, ab=ab: e.tensor_tensor(out=arg[ab][:], in0=arg[ab][:], in1=m2[ab][:],
                                                             op=ALU.add),
                     reads=[b_arg[ab], b_m[ab]], writes=[b_arg[ab]])
                S.op("act", lambda e, ab=ab, layer=layer, g=g: e.activation(
                    out=hid[layer][:, g * 512:(g + 1) * 512], in_=arg[ab][:], func=AF.Sin),
                    reads=[b_arg[ab]], writes=[b_hid[layer][g]])

        dbg("hid0", hid[0][:], [64, L], F32, b_hid[0])
        dbg("hid1", hid[1][:], [64, L], BF16, b_hid[1])
        fsd = sb(pf, "fsd", [128, 16, 2, 2, 512], BF16)
        b_fsd = bufs("fsd", 16)
        dfb = [sb(pf, f"dfb{i}", [128, 512], F32) for i in range(2)]
        dbb = [sb(pf, f"dbb{i}", [128, 512], F32) for i in range(2)]
        b_dec = bufs("dec", 2)
        Ft = [sb(pf, f"Ft{i}", [128, 512], F32) for i in range(2)]
        Bt = [sb(pf, f"Bt{i}", [128, 512], F32) for i in range(2)]
        b_FB = bufs("FB", 2)
        nfb = 0
        for tt in range(16):
            db_ = tt % 2
            S.dma("sp", lambda e, tt=tt, db_=db_: e.dma_start(out=dfb[db_][:], in_=decf_d[tt * 128:(tt + 1) * 128, :]),
                  writes=[b_dec[db_]])
            S.dma("sp", lambda e, tt=tt, db_=db_: e.dma_start(out=dbb[db_][:], in_=decb_d[tt * 128:(tt + 1) * 128, :]),
                  writes=[b_dec[db_]])
            for cg in range(4):
                S.op("pe", lambda e, tt=tt, cg=cg: e.matmul(
                    fps[2 + cg][:], lhsT=hid[1][:, tt * 128:(tt + 1) * 128], rhs=w3s[:, cg * 512:(cg + 1) * 512],
                    start=True, stop=True), reads=[b_hid[1][tt // 4], b_w3s], writes=[b_fps[2 + cg]])
            for o in range(2):
                k = nfb % 2
                nfb += 1

                def _fbm(e, o=o, k=k, db_=db_):
                    e.tensor_tensor(out=Ft[k][:], in0=fps[2 + o][:], in1=dfb[db_][:], op=ALU.mult)
                    return e.tensor_tensor(out=Bt[k][:], in0=fps[4 + o][:], in1=dbb[db_][:], op=ALU.mult)
                S.op("dve", _fbm, reads=[b_fps[2 + o], b_fps[4 + o], b_dec[db_]], writes=[b_FB[k]])

                def _sd(e, o=o, k=k, tt=tt):
                    e.tensor_tensor(out=fsd[:, tt, 0, o, :], in0=Ft[k][:], in1=Bt[k][:], op=ALU.add)
                    return e.tensor_tensor(out=fsd[:, tt, 1, o, :], in0=Ft[k][:], in1=Bt[k][:], op=ALU.subtract)
                S.op("pool", _sd, reads=[b_FB[k]], writes=[b_fsd[tt]])

        dbg("fsd", fsd[:].rearrange("p a b c d -> p (a b c d)"), [128, 16 * 2 * 2 * 512], BF16, b_fsd)
        cft = [sb(pf, f"cftF{i}", [128, 16, 128], BF16) for i in range(2)]
        sft = [sb(pf, f"sftF{i}", [128, 16, 128], BF16) for i in range(2)]
        b_cs = bufs("csF", 2)
        kt = [sb(pf, f"ktF{i}", [128, 2, 512], F32) for i in range(2)]
        b_kt = bufs("ktF", 2)
        nk = 0
        for w in range(16):
            wb = w % 2
            S.dma("sp", lambda e, w=w, wb=wb: e.dma_start(out=cft[wb][:].rearrange("p a b -> p (a b)"), in_=Cf_d[w]),
                  writes=[b_cs[wb]])
            S.dma("sp", lambda e, w=w, wb=wb: e.dma_start(out=sft[wb][:].rearrange("p a b -> p (a b)"), in_=Sf_d[w]),
                  writes=[b_cs[wb]])
            for o in range(2):
                for ab_ in range(2):
                    mat = cft if ab_ == 0 else sft

                    def _kmm(e, o=o, ab_=ab_, mat=mat, wb=wb):
                        ins = None
                        for tt in range(16):
                            ins = e.matmul(fps[2 + o * 2 + ab_][:], lhsT=mat[wb][:, tt, :], rhs=fsd[:, tt, ab_, o, :],
                                           start=(tt == 0), stop=(tt == 15))
                        return ins
                    S.op("pe", _kmm, reads=[b_cs[wb]] + b_fsd, writes=[b_fps[2 + o * 2 + ab_]])
                k = nk % 2
                nk += 1

                def _kev(e, o=o, k=k):
                    e.copy(out=kt[k][:, 0, :], in_=fps[2 + o * 2][:])
                    return e.copy(out=kt[k][:, 1, :], in_=fps[2 + o * 2 + 1][:])
                S.op("act", _kev, reads=[b_fps[2 + o * 2], b_fps[2 + o * 2 + 1]], writes=[b_kt[k]])
                S.dma("pool", lambda e, o=o, w=w, k=k: e.dma_start(
                    out=Kscr[o, w], in_=kt[k][:].rearrange("p a c -> p (a c)")),
                    reads=[b_kt[k]], writes=[b_K[o][w]])
        S.barrier()
    out_bufs += b_K[0] + b_K[1]
    if stop_after == "F":
        S.finish("sp", out_bufs)
        return nc, es

    b_Z1 = bufs("Z1_", NT)
    b_MIXh = bufs("MIXh", NT)
    with ExitStack() as ph:
        ph.enter_context(nc.named_scope("phH"))
        zbf = sb(ph, "zbf", [128, NSEQ, 16, 512], BF16)
        b_zbf = bufs("zbf", NT)
        PQ = sb(ph, "PQ", [128, 16, NSEQ, 2, 512], BF16)
        b_PQ = [bufs(f"PQ{b}_", 16) for b in range(NSEQ)]
        cft = [sb(ph, f"cftH{i}", [128, 16, 128], BF16) for i in range(2)]
        sft = [sb(ph, f"sftH{i}", [128, 16, 128], BF16) for i in range(2)]
        b_cs = bufs("csH", 2)
        ktile = [sb(ph, f"ktH{i}", [128, 2, 512], F32) for i in range(2)]
        b_ktile = bufs("ktH", 2)
        skb = sb(ph, "skb", [128, 2, 512], F32)
        ghy = sb(ph, "ghy", [128, 512], F32)
        b_hc = Buf("hconst")
        S.dma("sp", lambda e: e.dma_start(out=skb[:].rearrange("p a c -> p (a c)"),
                                          in_=skip_d.rearrange("a n -> (a n)").partition_broadcast(128)),
              writes=[b_hc])
        S.dma("sp", lambda e: e.dma_start(out=ghy[:], in_=ghy_d.rearrange("a n -> (a n)").partition_broadcast(128)),
              writes=[b_hc])
        hps = [ps(ph, f"hps{i}", [128, 512], F32) for i in range(8)]
        b_hps = bufs("hps", 8)
        NW = 3
        t1 = [sb(ph, f"t1_{i}", [128, 512], F32) for i in range(NW)]
        t2 = [sb(ph, f"t2_{i}", [128, 512], F32) for i in range(NW)]
        b_t = bufs("t12_", NW)
        gt = [sb(ph, f"gt{i}", [128, 512], F32) for i in range(NW)]
        zp = [sb(ph, f"zp{i}", [128, 512], F32) for i in range(NW)]
        b_gz = bufs("gz", NW)
        zn = [sb(ph, f"zn{i}", [128, 512], F32) for i in range(NW)]
        b_zn = bufs("zn", NW)
        mixt = [sb(ph, f"mixt{i}", [128, 512], BF16) for i in range(2)]
        b_mixt = bufs("mixt", 2)
        junkh = sb(ph, "junkh", [128, 512], BF16)
        b_junkh = Buf("junkh")
        sq = sb(ph, "sqh", [128, NT], F32)
        sr = sb(ph, "srh", [128, NT], F32)
        srr = sb(ph, "srrh", [128, NT], F32)
        b_sq = bufs("sqh", NT)

        S.dma("pool", lambda e: e.dma_start(
            out=zbf[:].rearrange("p b t c -> p (b t) c"),
            in_=U_hy.rearrange("(j p) c -> p j c", p=128)[:, :, 2 * HYW:3 * HYW]),
            reads=b_Uhy[2], writes=b_zbf)

        nt_ = 0
        for o in range(2):
            for w in range(16):
                wb = w % 2
                S.dma("sp", lambda e, w=w, wb=wb: e.dma_start(out=cft[wb][:].rearrange("p a b -> p (a b)"), in_=Cf_d[w]),
                      writes=[b_cs[wb]])
                S.dma("sp", lambda e, w=w, wb=wb: e.dma_start(out=sft[wb][:].rearrange("p a b -> p (a b)"), in_=Sf_d[w]),
                      writes=[b_cs[wb]])
                S.dma("sp", lambda e, o=o, w=w, wb=wb: e.dma_start(out=ktile[wb][:].rearrange("p a c -> p (a c)"),
                                                                 in_=Kscr[o, w]),
                      reads=[b_K[o][w]], writes=[b_ktile[wb]])
                for b in range(NSEQ):
                    pa_, pb_ = wb * 4 + b * 2, wb * 4 + b * 2 + 1
                    for ab_, bank in ((0, pa_), (1, pb_)):
                        mat = cft if ab_ == 0 else sft

                        def _fmm(e, mat=mat, wb=wb, b=b, bank=bank):
                            ins = None
                            for tt in range(16):
                                ins = e.matmul(hps[bank][:], lhsT=mat[wb][:, tt, :], rhs=zbf[:, b, tt, :],
                                               start=(tt == 0), stop=(tt == 15))
                            return ins
                        S.op("pe", _fmm, reads=[b_cs[wb]] + b_zbf[b * 16:(b + 1) * 16], writes=[b_hps[bank]])
                    for pq in range(2):
                        k = nt_ % NW
                        nt_ += 1
                        ka, kb = (0, 1) if pq == 0 else (1, 0)

                        def _pr(e, k=k, pa_=pa_, pb_=pb_, wb=wb, ka=ka, kb=kb):
                            e.tensor_tensor(out=t1[k][:], in0=hps[pa_][:], in1=ktile[wb][:, ka, :], op=ALU.mult)
                            return e.tensor_tensor(out=t2[k][:], in0=hps[pb_][:], in1=ktile[wb][:, kb, :], op=ALU.mult)
                        S.op("dve", _pr, reads=[b_hps[pa_], b_hps[pb_], b_ktile[wb]], writes=[b_t[k]])
                        S.op("pool", lambda e, k=k, w=w, b=b, pq=pq: e.tensor_tensor(
                            out=PQ[:, w, b, pq, :], in0=t1[k][:], in1=t2[k][:],
                            op=(ALU.subtract if pq == 0 else ALU.add)),
                            reads=[b_t[k]], writes=[b_PQ[b][w]])
            for tt in range(16):
                tb = tt % 2
                S.dma("sp", lambda e, tt=tt, tb=tb: e.dma_start(out=cft[tb][:].rearrange("p a b -> p (a b)"), in_=Ci_d[tt]),
                      writes=[b_cs[tb]])
                S.dma("sp", lambda e, tt=tt, tb=tb: e.dma_start(out=sft[tb][:].rearrange("p a b -> p (a b)"), in_=Si_d[tt]),
                      writes=[b_cs[tb]])
                for b in range(NSEQ):
                    j = b * 16 + tt
                    bank = (tt * 2 + b) % 8

                    def _imm(e, tb=tb, b=b, bank=bank):
                        ins = None
                        n = 0
                        for w in range(16):
                            for pq, mat in ((0, cft), (1, sft)):
                                ins = e.matmul(hps[bank][:], lhsT=mat[tb][:, w, :], rhs=PQ[:, w, b, pq, :],
                                               start=(n == 0), stop=(n == 31))
                                n += 1
                        return ins
                    S.op("pe", _imm, reads=[b_cs[tb]] + b_PQ[b], writes=[b_hps[bank]])
                    k = nt_ % NW
                    nt_ += 1
                    S.dma("sp", lambda e, j=j, o=o, k=k: e.dma_start(
                        out=gt[k][:], in_=U_hy[j * 128:(j + 1) * 128, o * HYW:(o + 1) * HYW]),
                        reads=[b_Uhy[o][j]], writes=[b_gz[k]])
                    if o == 0:
                        S.dma("sp", lambda e, j=j, k=k: e.dma_start(
                            out=zp[k][:], in_=U_hy[j * 128:(j + 1) * 128, 2 * HYW:3 * HYW]),
                            reads=[b_Uhy[2][j]], writes=[b_gz[k]])
                    else:
                        S.dma("sp", lambda e, j=j, k=k: e.dma_start(out=zp[k][:], in_=Z1[j * 128:(j + 1) * 128, :]),
                              reads=[b_Z1[j]], writes=[b_gz[k]])
                    S.op("pool", lambda e, k=k, o=o: e.tensor_tensor(out=t1[k][:], in0=zp[k][:], in1=skb[:, o, :],
                                                                     op=ALU.mult),
                         reads=[b_gz[k], b_hc], writes=[b_t[k]])
                    S.op("dve", lambda e, k=k, bank=bank: e.tensor_tensor(out=t2[k][:], in0=hps[bank][:], in1=t1[k][:],
                                                                          op=ALU.add),
                         reads=[b_hps[bank], b_t[k]], writes=[b_t[k]])
                    S.op("dve", lambda e, k=k: e.tensor_tensor(out=zn[k][:], in0=t2[k][:], in1=gt[k][:], op=ALU.mult),
                         reads=[b_t[k], b_gz[k]], writes=[b_zn[k]])
                    if o == 0:
                        S.dma("pool", lambda e, j=j, k=k: e.dma_start(out=Z1[j * 128:(j + 1) * 128, :], in_=zn[k][:]),
                              reads=[b_zn[k]], writes=[b_Z1[j]])
                        S.op("act", lambda e, k=k, b=b, tt=tt: e.copy(out=zbf[:, b, tt, :], in_=zn[k][:]),
                             reads=[b_zn[k]], writes=[b_zbf[j]])
                    else:
                        S.op("act", lambda e, k=k, j=j: e.activation(
                            out=junkh[:], in_=zn[k][:], func=AF.Square, accum_out=sq[:, j:j + 1]),
                            reads=[b_zn[k]], writes=[b_junkh, b_sq[j]])
                        S.op("act", lambda e, j=j: e.activation(
                            out=sr[:, j:j + 1], in_=sq[:, j:j + 1], func=AF.Sqrt, scale=1.0 / HYW, bias=EPS),
                            reads=[b_sq[j]], writes=[b_sq[j]])
                        S.op("dve", lambda e, j=j: e.reciprocal(out=srr[:, j:j + 1], in_=sr[:, j:j + 1]),
                             reads=[b_sq[j]], writes=[b_sq[j]])
                        mb = j % 2
                        S.op("dve", lambda e, k=k, j=j, mb=mb: e.scalar_tensor_tensor(
                            out=mixt[mb][:], in0=zn[k][:], scalar=srr[:, j:j + 1], in1=ghy[:],
                            op0=ALU.mult, op1=ALU.mult),
                            reads=[b_zn[k], b_sq[j], b_hc], writes=[b_mixt[mb]])
                        S.dma("pool", lambda e, j=j, mb=mb: e.dma_start(
                            out=MIX[j * 128:(j + 1) * 128, 0:HYW], in_=mixt[mb][:]),
                            reads=[b_mixt[mb]], writes=[b_MIXh[j]])
        S.barrier()
    out_bufs += b_Z1 + b_MIXh
    if stop_after == "H":
        S.finish("sp", out_bufs)
        return nc, es

    b_MIXn = bufs("MIXn", NT)
    with ExitStack() as pn:
        pn.enter_context(nc.named_scope("phN"))
        nmask = sb(pn, "nmask", [128, 5, 640], F32)
        gna = sb(pn, "gna", [128, 512], F32)
        b_nc = Buf("nconst")
        b_nc2 = Buf("nconst2")
        S.dma("sp", lambda e: e.dma_start(out=nmask[:].rearrange("p a c -> p (a c)"), in_=nmask_d[:, :]), writes=[b_nc])
        S.dma("sp", lambda e: e.dma_start(out=gna[:], in_=gna_d.rearrange("a n -> (a n)").partition_broadcast(128)),
              writes=[b_nc2])
        rpbT = [sb(pn, f"rpbT{i}", [128, 18 * 64], F32) for i in range(2)]
        b_rpbT = bufs("rpbT", 2)
        CB = [sb(pn, f"CB{i}", [128, 5, 640], F32) for i in range(2)]
        b_CB = bufs("CB", 2)
        qTs = [sb(pn, f"qTs{i}", [64, L], BF16) for i in range(2)]
        kTs = [sb(pn, f"kTs{i}", [64, L], BF16) for i in range(2)]
        b_qk = bufs("qk", 2)
        b_qk2 = bufs("qk2_", 2)
        vA = [sb(pn, f"vA{i}", [128, 16, NAH * 65], BF16) for i in range(2)]
        b_vA = bufs("vA", 2)
        ytile = [sb(pn, f"ytile{i}", [128, 16, 512], F32) for i in range(2)]
        b_yt = [bufs(f"yt{i}_", 16) for i in range(2)]
        sps = [ps(pn, f"sps{i}", [128, 1024], F32) for i in range(2)]
        b_sps = bufs("sps", 2)
        ops_ = [ps(pn, f"ops{i}", [128, 512], F32) for i in range(2)]
        b_ops = bufs("ops", 2)
        ssb = [sb(pn, f"ssb{i}", [128, 640], F32) for i in range(2)]
        b_ssb = bufs("ssb", 2)
        pTs = [sb(pn, f"pTs{i}", [128, 640], BF16) for i in range(5)]
        b_pTs = bufs("pTs", 5)
        rc = sb(pn, "rcn", [128, 4], F32)
        b_rc = bufs("rcn", 4)
        junkn = sb(pn, "junkn", [128, 512], BF16)
        b_junkn = Buf("junkn")
        sqn = sb(pn, "sqn", [128, NT], F32)
        srn = sb(pn, "srn", [128, NT], F32)
        srrn = sb(pn, "srrn", [128, NT], F32)
        b_sqn = bufs("sqn", NT)
        mixn = [sb(pn, f"mixn{i}", [128, 512], BF16) for i in range(2)]
        b_mixn = bufs("mixn", 2)
        TYPE_OF = {0: 0, 1: 1, 14: 3, 15: 4}
        OFF_OF = {0: 0, 1: 2, 2: 4, 3: 6, 4: 8}
        for b in range(NSEQ):
            vb = b % 2
            S.dma("sp", lambda e, b=b, vb=vb: e.dma_start(
                out=vA[vb][:], in_=VA.rearrange("(j p) c -> p j c", p=128)[:, b * 16:(b + 1) * 16, :]),
                reads=b_VA[b * 16:(b + 1) * 16], writes=[b_vA[vb]])
            items = [(h, p) for h in range(NAH) for p in range(16)]

            def n_st0(idx, b=b, vb=vb):
                h, p = items[idx]
                it = b * len(items) + idx
                hb = (b * NAH + h) % 2
                if p == 0:
                    S.dma("sp", lambda e: e.dma_start(out=rpbT[hb][:], in_=rpbT_d[h]), writes=[b_rpbT[hb]])
                    S.dma("sp", lambda e: e.dma_start(
                        out=qTs[hb][:], in_=QT[h // 2, (h % 2) * 64:(h % 2) * 64 + 64, b * L:(b + 1) * L]),
                        reads=[b_QT], writes=[b_qk[hb]])
                    S.dma("sp", lambda e: e.dma_start(
                        out=kTs[hb][:], in_=KT[h // 2, (h % 2) * 64:(h % 2) * 64 + 64, b * L:(b + 1) * L]),
                        reads=[b_KT], writes=[b_qk2[hb]])

                    def _cb(e):
                        ins = None
                        for ty_ in range(5):
                            off = OFF_OF[ty_] * 64
                            ins = e.tensor_tensor(out=CB[hb][:, ty_, :], in0=rpbT[hb][:, off:off + 640],
                                                  in1=nmask[:, ty_, :], op=ALU.add)
                        return ins
                    S.op("pool", _cb, reads=[b_rpbT[hb], b_nc], writes=[b_CB[hb]])
                P0 = min(max(p - 2, 0), 11)
                ty = TYPE_OF.get(p, 2)
                sb_i = it % 2
                pt_i = it % 5

                def _smm(e):
                    ins = None
                    for jp in range(5):
                        kt_ = P0 + 4 - jp
                        ins = e.matmul(sps[sb_i][:, jp * 128:(jp + 1) * 128],
                                       lhsT=kTs[hb][:, kt_ * 128:(kt_ + 1) * 128],
                                       rhs=qTs[hb][:, p * 128:(p + 1) * 128], start=True, stop=True)
                    return ins
                S.op("pe", _smm, reads=[b_qk[hb], b_qk2[hb]], writes=[b_sps[sb_i]])
                S.op("dve", lambda e: e.tensor_tensor(
                    out=ssb[sb_i][:], in0=sps[sb_i][:, 0:640], in1=CB[hb][:, ty, :], op=ALU.add),
                    reads=[b_sps[sb_i], b_CB[hb]], writes=[b_ssb[sb_i]])
                S.op("act", lambda e: e.activation(out=pTs[pt_i][:], in_=ssb[sb_i][:], func=AF.Exp),
                     reads=[b_ssb[sb_i]], writes=[b_pTs[pt_i]])

            def n_st1(idx, b=b, vb=vb):
                h, p = items[idx]
                it = b * len(items) + idx
                P0 = min(max(p - 2, 0), 11)
                o_i = it % 2
                pt_i = it % 5
                ri = it % 4

                def _omm(e):
                    ins = None
                    for jp in range(5):
                        kt_ = P0 + 4 - jp
                        ins = e.matmul(ops_[o_i][:, 0:65], lhsT=pTs[pt_i][:, jp * 128:(jp + 1) * 128],
                                       rhs=vA[vb][:, kt_, h * 65:(h + 1) * 65], start=(jp == 0), stop=(jp == 4))
                    return ins
                S.op("pe", _omm, reads=[b_pTs[pt_i], b_vA[vb]], writes=[b_ops[o_i]])
                S.op("dve", lambda e: e.reciprocal(out=rc[:, ri:ri + 1], in_=ops_[o_i][:, 64:65]),
                     reads=[b_ops[o_i]], writes=[b_rc[ri]])
                S.op("act", lambda e: e.activation(
                    out=ytile[vb][:, p, h * 64:(h + 1) * 64], in_=ops_[o_i][:, 0:64], func=AF.Copy,
                    scale=rc[:, ri:ri + 1]),
                    reads=[b_ops[o_i], b_rc[ri]], writes=[b_yt[vb][p]])
            pipeline(len(items), [n_st0, n_st1], [0, 3])
            for p in range(16):
                j = b * 16 + p
                S.op("act", lambda e, vb=vb, p=p, j=j: e.activation(
                    out=junkn[:], in_=ytile[vb][:, p, :], func=AF.Square, accum_out=sqn[:, j:j + 1]),
                    reads=[b_yt[vb][p]], writes=[b_junkn, b_sqn[j]])
                S.op("act", lambda e, j=j: e.activation(
                    out=srn[:, j:j + 1], in_=sqn[:, j:j + 1], func=AF.Sqrt, scale=1.0 / HYW, bias=EPS),
                    reads=[b_sqn[j]], writes=[b_sqn[j]])
                S.op("dve", lambda e, j=j: e.reciprocal(out=srrn[:, j:j + 1], in_=srn[:, j:j + 1]),
                     reads=[b_sqn[j]], writes=[b_sqn[j]])
                mb = j % 2
                S.op("dve", lambda e, vb=vb, p=p, j=j, mb=mb: e.scalar_tensor_tensor(
                    out=mixn[mb][:], in0=ytile[vb][:, p, :], scalar=srrn[:, j:j + 1], in1=gna[:],
                    op0=ALU.mult, op1=ALU.mult),
                    reads=[b_yt[vb][p], b_sqn[j], b_nc2], writes=[b_mixn[mb]])
                S.dma("pool", lambda e, j=j, mb=mb: e.dma_start(
                    out=MIX[j * 128:(j + 1) * 128, HYW:2 * HYW], in_=mixn[mb][:]),
                    reads=[b_mixn[mb]], writes=[b_MIXn[j]])
        S.barrier()
    out_bufs += b_MIXn
    if stop_after == "N":
        S.finish("sp", out_bufs)
        return nc, es

    b_H1 = bufs("H1_", NT)
    b_XSw = bufs("XSw", NT)
    with ExitStack() as po:
        po.enter_context(nc.named_scope("phO"))
        woutb = sb(po, "woutb", [128, 8, D], BF16)
        b_wout = Buf("wout")
        S.dma("pool", lambda e: e.dma_start(out=woutb[:], in_=w_out_d.rearrange("(c p) n -> p c n", p=128)),
              writes=[b_wout])
        gffn = sb(po, "gffn", [128, D], F32)
        wr = sb(po, "wr", [128, 8, 36], F32)
        brb = sb(po, "brb", [128, 36], F32)
        ust = sb(po, "ust", [128, 128], F32)
        onesf = sb(po, "onesf", [128, 128], F32)
        ecap = sb(po, "ecap", [128, NE], F32)
        ecapu = sb(po, "ecapu", [128, NE], F32)
        ocum = sb(po, "ocum", [128, NE], F32)
        b_oc = [Buf(f"oconst{i}") for i in range(7)]
        S.dma("sp", lambda e: e.dma_start(out=gffn[:], in_=gffn_d.rearrange("a n -> (a n)").partition_broadcast(128)),
              writes=[b_oc[0]])
        S.dma("sp", lambda e: e.dma_start(out=wr[:], in_=wr_d[:, :, :]), writes=[b_oc[1]])
        S.dma("sp", lambda e: e.dma_start(out=brb[:], in_=br_d.rearrange("a n -> (a n)").partition_broadcast(128)),
              writes=[b_oc[2]])
        S.dma("sp", lambda e: e.dma_start(out=ust[:], in_=ust_d[:, :]), writes=[b_oc[3]])
        S.dma("sp", lambda e: e.dma_start(out=onesf[:], in_=onesf_d[:, :]), writes=[b_oc[4]])
        S.dma("sp", lambda e: e.dma_start(out=ecap[:], in_=ecap_d.rearrange("a n -> (a n)").partition_broadcast(128)),
              writes=[b_oc[5]])
        S.op("dve", lambda e: e.tensor_scalar(out=ecapu[:], in0=ecap[:], scalar1=float(CAP - 1), scalar2=None,
                                              op0=ALU.add), reads=[b_oc[5]], writes=[b_oc[6]])
        b_ocum = Buf("ocum")
        S.op("pool", lambda e: e.memset(ocum[:], 0.0), writes=[b_ocum])

        NB = 6
        NR = 5
        mx = [sb(po, f"mx{i}", [128, D], BF16) for i in range(2)]
        b_mx = bufs("mx", 2)
        mxT = [sb(po, f"mxT{i}", [128, 8, 128], BF16) for i in range(3)]
        b_mxT = bufs("mxT", 3)
        xo = [sb(po, f"xo{i}", [128, D], F32) for i in range(3)]
        b_xo = bufs("xo", 3)
        h1t = [sb(po, f"h1t{i}", [128, D], F32) for i in range(3)]
        b_h1t = bufs("h1t", 3)
        xg = [sb(po, f"xg{i}", [128, D], F32) for i in range(3)]
        b_xg = bufs("xg", 3)
        xgb = [sb(po, f"xgb{i}", [128, D], BF16) for i in range(NB)]
        b_xgb = bufs("xgb", NB)
        xgT = [sb(po, f"xgT{i}", [128, 8, 128], F32) for i in range(2)]
        b_xgT = bufs("xgT", 2)
        junko = sb(po, "junko", [128, D], BF16)
        b_junko = Buf("junko")
        st = sb(po, "sto", [128, NT, 6], F32)
        b_st = bufs("sto", NT)
        lg = [sb(po, f"lg{i}", [128, 36], F32) for i in range(NR)]
        sm = [sb(po, f"sm{i}", [128, 16], F32) for i in range(NR)]
        ohg = [sb(po, f"ohg{i}", [128, 4, 1], F32) for i in range(NR)]
        ge = [sb(po, f"ge{i}", [128, 4], F32) for i in range(NR)]
        tmp48 = [sb(po, f"tmp48{i}", [128, 4, 8], F32) for i in range(NR)]
        el8 = [sb(po, f"el8{i}", [128, 8], F32) for i in range(NR)]
        m8 = [sb(po, f"m8{i}", [128, 8], F32) for i in range(NR)]
        ohk = [[sb(po, f"ohk{i}_{k}", [128, 1, 8], F32) for k in range(2)] for i in range(NR)]
        Ok = [[sb(po, f"Ok{i}_{k}", [128, 4, 8], F32) for k in range(2)] for i in range(NR)]
        Osum = sb(po, "Osum", [128, NT, NE], BF16)
        ustb = sb(po, "ustb", [128, 128], BF16)
        onesb = sb(po, "onesb", [128, 128], BF16)
        ocumb = [sb(po, f"ocumb{i}", [128, NE], BF16) for i in range(2)]
        b_ocumb = bufs("ocumb", 2)
        S.op("pool", lambda e: e.memset(ocumb[0][:], 0.0), writes=[b_ocumb[0]])
        S.op("act", lambda e: e.copy(out=ustb[:], in_=ust[:]), reads=[b_oc[3]], writes=[b_oc[3]])
        S.op("act", lambda e: e.copy(out=onesb[:], in_=onesf[:]), reads=[b_oc[4]], writes=[b_oc[4]])
        b_Osum = bufs("Osum", NT)
        slot = [sb(po, f"slot{i}", [128, NE], F32) for i in range(NR)]
        tmp32 = [sb(po, f"tmp32{i}", [128, NE], F32) for i in range(NR)]
        dk = [sb(po, f"dk{i}", [128, 4], F32) for i in range(NR)]
        b_rt = bufs("rtile", NR)
        tpb = [ps(po, f"tpb{i}", [128, 8, 128], BF16) for i in range(1)]
        b_tpb = bufs("tpb", 1)
        tpf = [ps(po, f"tpf{i}", [128, 8, 128], F32) for i in range(1)]
        b_tpf = bufs("tpf", 1)
        accO = [ps(po, f"accO{i}", [128, 512], F32) for i in range(2)]
        b_accO = bufs("accO", 2)
        rpsLt = [ps(po, f"rpsL{i}", [128, 512], F32) for i in range(2)]
        rpsRt = ps(po, "rpsR", [128, 512], F32)
        b_rpsL = bufs("rpsL", 2)
        b_rpsR1 = Buf("rpsR")
        fl = lambda a: a.rearrange("p g x -> p (g x)")

        def o_st0(j):
            S.dma("sp", lambda e: e.dma_start(out=mx[j % 2][:], in_=MIX[j * 128:(j + 1) * 128, :]),
                  reads=[b_MIXh[j], b_MIXn[j]], writes=[b_mx[j % 2]])
            S.dma("sp", lambda e: e.dma_start(out=xo[j % 3][:], in_=x[j * 128:(j + 1) * 128, :]),
                  writes=[b_xo[j % 3]])

            def _tr(e):
                ins = None
                for c in range(8):
                    ins = e.transpose(out=tpb[0][:, c, :], in_=mx[j % 2][:, c * 128:(c + 1) * 128],
                                      identity=ident_bf[:])
                return ins
            S.op("pe", _tr, reads=[b_mx[j % 2], b_ident], writes=[b_tpb[0]])
            S.op("act", lambda e: e.copy(out=mxT[j % 3][:], in_=tpb[0][:]), reads=[b_tpb[0]], writes=[b_mxT[j % 3]])

        def o_st1(j):
            i3 = j % 3
            for half in range(2):
                a = half

                def _mm(e, half=half, a=a):
                    ins = None
                    for c in range(8):
                        ins = e.matmul(accO[a][:], lhsT=mxT[i3][:, c, :], rhs=woutb[:, c, half * 512:(half + 1) * 512],
                                       start=(c == 0), stop=(c == 7))
                    return ins
                S.op("pe", _mm, reads=[b_mxT[i3], b_wout], writes=[b_accO[a]])
                S.op("dve", lambda e, half=half, a=a: e.tensor_tensor(
                    out=h1t[i3][:, half * 512:(half + 1) * 512], in0=accO[a][:],
                    in1=xo[i3][:, half * 512:(half + 1) * 512], op=ALU.add),
                    reads=[b_accO[a], b_xo[i3]], writes=[b_h1t[i3]])
            S.dma("pool", lambda e: e.dma_start(out=H1[j * 128:(j + 1) * 128, :], in_=h1t[i3][:]),
                  reads=[b_h1t[i3]], writes=[b_H1[j]])
            S.op("act", lambda e: e.activation(out=junko[:], in_=h1t[i3][:], func=AF.Square,
                                               accum_out=st[:, j, 0:1]),
                 reads=[b_h1t[i3]], writes=[b_junko, b_st[j]])

        def o_st1b(j):
            i3 = j % 3
            rsqrt_dve(st[:, j, 0:6], 1.0 / D, [b_st[j]])
            S.op("dve", lambda e: e.scalar_tensor_tensor(
                out=xg[i3][:], in0=h1t[i3][:], scalar=st[:, j, 2:3], in1=gffn[:], op0=ALU.mult, op1=ALU.mult),
                reads=[b_h1t[i3], b_st[j], b_oc[0]], writes=[b_xg[i3]])
            S.op("act", lambda e: e.copy(out=xgb[j % NB][:], in_=xg[i3][:]), reads=[b_xg[i3]], writes=[b_xgb[j % NB]])

        def o_st2(j):
            i3 = j % 3
            i2 = j % 2
            q4 = j % 4

            def _trf(e):
                ins = None
                for c in range(8):
                    ins = e.transpose(out=tpf[0][:, c, :], in_=xg[i3][:, c * 128:(c + 1) * 128], identity=ident_f[:])
                return ins
            S.op("pe", _trf, reads=[b_xg[i3], b_ident], writes=[b_tpf[0]])
            S.op("act", lambda e: e.copy(out=xgT[i2][:], in_=tpf[0][:]), reads=[b_tpf[0]], writes=[b_xgT[i2]])

            def _rmm(e):
                ins = None
                for c in range(8):
                    ins = e.matmul(rpsLt[j % 2][:, 0:36], lhsT=xgT[i2][:, c, :], rhs=wr[:, c, :],
                                   start=(c == 0), stop=(c == 7))
                return ins
            S.op("pe", _rmm, reads=[b_xgT[i2], b_oc[1]], writes=[b_rpsL[j % 2]])

        def o_st3(j):
            i = j % NR
            q4 = j % 4
            S.chain("dve", [
                lambda e: e.tensor_tensor(out=lg[i][:], in0=rpsLt[j % 2][:, 0:36], in1=brb[:], op=ALU.add),
                lambda e: e.tensor_reduce(out=sm[i][:, 0:1], in_=lg[i][:, 0:4], axis=AX.X, op=ALU.max),
                lambda e: e.tensor_scalar(out=sm[i][:, 1:2], in0=sm[i][:, 0:1], scalar1=-1.0, scalar2=None,
                                          op0=ALU.mult),
                lambda e: e.tensor_scalar(out=ohg[i][:, :, 0], in0=lg[i][:, 0:4], scalar1=sm[i][:, 0:1],
                                          scalar2=None, op0=ALU.is_equal),
            ], reads=[b_rpsL[j % 2], b_oc[2]], writes=[b_rt[i]])
            S.op("act", lambda e: e.activation(out=ge[i][:], in_=lg[i][:, 0:4], func=AF.Exp,
                                               bias=sm[i][:, 1:2], accum_out=sm[i][:, 2:3]),
                 reads=[b_rt[i]], writes=[b_rt[i]])

        def o_st3b(j):
            i = j % NR
            S.chain("dve", [
                lambda e: e.reciprocal(out=sm[i][:, 3:4], in_=sm[i][:, 2:3]),
                lambda e: e.tensor_tensor(out=tmp48[i][:], in0=lg[i][:, 4:36].rearrange("p (g x) -> p g x", g=4),
                                          in1=ohg[i][:].broadcast_to([128, 4, 8]), op=ALU.mult),
                lambda e: e.tensor_reduce(out=el8[i][:], in_=tmp48[i][:].rearrange("p g x -> p x g"),
                                          axis=AX.X, op=ALU.add),
                lambda e: e.max(out=m8[i][:], in_=el8[i][:]),
                lambda e: e.tensor_scalar(out=ohk[i][0][:, 0, :], in0=el8[i][:], scalar1=m8[i][:, 0:1],
                                          scalar2=None, op0=ALU.is_equal),
                lambda e: e.tensor_scalar(out=ohk[i][1][:, 0, :], in0=el8[i][:], scalar1=m8[i][:, 1:2],
                                          scalar2=None, op0=ALU.is_equal),
                lambda e: e.tensor_scalar(out=sm[i][:, 4:5], in0=m8[i][:, 0:1], scalar1=-1.0, scalar2=None,
                                          op0=ALU.mult),
            ], reads=[b_rt[i]], writes=[b_rt[i]])
            S.op("act", lambda e: e.activation(out=sm[i][:, 5:6], in_=m8[i][:, 1:2], func=AF.Exp,
                                               bias=sm[i][:, 4:5]),
                 reads=[b_rt[i]], writes=[b_rt[i]])

        def o_st3c(j):
            i = j % NR
            S.chain("dve", [
                lambda e: e.tensor_scalar(out=sm[i][:, 6:7], in0=sm[i][:, 5:6], scalar1=1.0, scalar2=None,
                                          op0=ALU.add),
                lambda e: e.reciprocal(out=sm[i][:, 7:8], in_=sm[i][:, 6:7]),
                lambda e: e.tensor_tensor(out=gates[:, j, 0:1], in0=sm[i][:, 7:8], in1=sm[i][:, 3:4], op=ALU.mult),
                lambda e: e.tensor_tensor(out=gates[:, j, 1:2], in0=gates[:, j, 0:1], in1=sm[i][:, 5:6],
                                          op=ALU.mult),
                lambda e: e.tensor_tensor(out=Ok[i][0][:], in0=ohg[i][:].broadcast_to([128, 4, 8]),
                                          in1=ohk[i][0][:].broadcast_to([128, 4, 8]), op=ALU.mult),
                lambda e: e.tensor_tensor(out=Ok[i][1][:], in0=ohg[i][:].broadcast_to([128, 4, 8]),
                                          in1=ohk[i][1][:].broadcast_to([128, 4, 8]), op=ALU.mult),
                lambda e: e.tensor_tensor(out=Osum[:, j, :], in0=fl(Ok[i][0][:]), in1=fl(Ok[i][1][:]), op=ALU.add),
            ], reads=[b_rt[i]], writes=[b_rt[i], b_route[j], b_Osum[j]])

        def o_st4(j):
            i = j % NR
            q4 = j % 4
            reg = rpsRt[:, 0:NE]

            def _rank(e):
                e.matmul(reg, lhsT=ustb[:], rhs=Osum[:, j, :], start=True, stop=False)
                return e.matmul(reg, lhsT=onesb[:], rhs=ocumb[j % 2][:], start=False, stop=True)
            S.op("pe", _rank, reads=[b_Osum[j], b_ocumb[j % 2], b_oc[3], b_oc[4]], writes=[b_rpsR1])
            S.op("pool", lambda e: e.tensor_tensor(out=ocumb[(j + 1) % 2][:], in0=ocumb[j % 2][:], in1=Osum[:, j, :],
                                                   op=ALU.add),
                 reads=[b_ocumb[j % 2], b_Osum[j]], writes=[b_ocumb[(j + 1) % 2]])
            dfns = [lambda e: e.tensor_tensor(out=slot[i][:], in0=reg, in1=ecap[:], op=ALU.add)]
            for k in range(2):
                dfns += [
                    lambda e, k=k: e.tensor_tensor(out=tmp32[i][:], in0=slot[i][:], in1=fl(Ok[i][k][:]), op=ALU.mult),
                    lambda e, k=k: e.tensor_reduce(out=dk[i][:, k:k + 1], in_=tmp32[i][:], axis=AX.X, op=ALU.add),
                    lambda e, k=k: e.tensor_tensor(out=tmp32[i][:], in0=ecapu[:], in1=fl(Ok[i][k][:]), op=ALU.mult),
                    lambda e, k=k: e.tensor_reduce(out=dk[i][:, 2 + k:3 + k], in_=tmp32[i][:], axis=AX.X, op=ALU.add),
                ]
            dfns += [
                lambda e: e.tensor_tensor(out=dk[i][:, 0:2], in0=dk[i][:, 0:2], in1=dk[i][:, 2:4], op=ALU.min),
                lambda e: e.tensor_copy(out=desti[:, j, :], in_=dk[i][:, 0:2]),
            ]
            S.chain("dve", dfns, reads=[b_rpsR1, b_rt[i], b_oc[5], b_oc[6]], writes=[b_rt[i], b_route[j]])
            for k in range(2):
                S.dma("pool", lambda e, k=k: e.indirect_dma_start(
                    out=XS[:, :], out_offset=bass.IndirectOffsetOnAxis(ap=desti[:, j, k:k + 1], axis=0),
                    in_=xgb[j % NB][:], in_offset=None), reads=[b_xgb[j % NB], b_route[j]] + b_XSall,
                    writes=[b_XSw[j]])
        pipeline(NT, [o_st0, o_st1, o_st1b, o_st2, o_st3, o_st3b, o_st3c, o_st4], [0, 1, 2, 3, 4, 5, 6, 7])
        dbg("desti", desti[:].rearrange("p t k -> p (t k)"), [128, NT * 2], I32, b_route)
        dbg("gates", gates[:].rearrange("p t k -> p (t k)"), [128, NT * 2], F32, b_route)
        S.barrier()
    out_bufs += b_H1 + b_XSw
    if stop_after == "O":
        S.finish("sp", out_bufs)
        return nc, es

    b_YS = bufs("YS", NE * 4)
    with ExitStack() as pe_:
        pe_.enter_context(nc.named_scope("phE"))
        w1b = [sb(pe_, f"w1b{i}", [128, 8, DEXP], BF16) for i in range(2)]
        w3b = [sb(pe_, f"w3b{i}", [128, 8, DEXP], BF16) for i in range(2)]
        w2b = [sb(pe_, f"w2b{i}", [128, 4, D], BF16) for i in range(3)]
        b_w1 = bufs("w1b", 2)
        b_w3 = bufs("w3b", 2)
        b_w2 = bufs("w2b", 3)
        NXS = 6
        xs = [sb(pe_, f"xs{i}", [128, D], BF16) for i in range(NXS)]
        b_xs = bufs("xs", NXS)
        xT = [sb(pe_, f"xTe{i}", [128, 8, CAP], BF16) for i in range(2)]
        b_xT = [bufs(f"xTe{i}_", 4) for i in range(2)]
        s1 = [sb(pe_, f"s1_{i}", [128, CAP], F32) for i in range(2)]
        b_s1 = bufs("s1_", 2)
        hT = [sb(pe_, f"hT{i}", [128, 4, CAP], BF16) for i in range(2)]
        b_hT = [bufs(f"hT{i}_", 4) for i in range(2)]
        yb = [sb(pe_, f"yb{i}", [128, D], F32) for i in range(4)]
        b_yb = bufs("yb", 4)
        b_ybh = [bufs(f"ybh{i}_", 2) for i in range(4)]
        tpe = [ps(pe_, f"tpe{i}", [128, 8, 128], BF16) for i in range(2)]
        b_tpe = bufs("tpe", 2)
        hps_ = [ps(pe_, f"hpsE{i}", [128, 512], F32) for i in range(4)]
        b_hpsE = bufs("hpsE", 4)
        yps = [ps(pe_, f"ypsE{i}", [128, 512], F32) for i in range(2)]
        b_yps = bufs("ypsE", 2)

        def e_st0(ex, blk):
            wb = ex % 2
            if blk == 0:
                S.dma("pool", lambda e: e.dma_start(
                    out=w1b[wb][:], in_=ew1_d[ex].rearrange("(c p) f -> p c f", p=128)), writes=[b_w1[wb]])
                S.dma("pool", lambda e: e.dma_start(
                    out=w3b[wb][:], in_=ew3_d[ex].rearrange("(c p) f -> p c f", p=128)), writes=[b_w3[wb]])
                S.dma("pool", lambda e: e.dma_start(
                    out=w2b[ex % 3][:], in_=ew2_d[ex].rearrange("(c p) f -> p c f", p=128)), writes=[b_w2[ex % 3]])
                for bb in range(4):
                    n_ = ex * 4 + bb
                    S.dma("sp", lambda e, n_=n_: e.dma_start(out=xs[n_ % NXS][:], in_=XS[n_ * 128:n_ * 128 + 128, :]),
                          reads=b_XSw + b_XSall, writes=[b_xs[n_ % NXS]])
            n = ex * 4 + blk
            xi = n % NXS
            ti = n % 2

            def _tr(e):
                ins = None
                for c in range(8):
                    ins = e.transpose(out=tpe[ti][:, c, :], in_=xs[xi][:, c * 128:(c + 1) * 128],
                                      identity=ident_bf[:])
                return ins
            S.op("pe", _tr, reads=[b_xs[xi], b_ident], writes=[b_tpe[ti]])
            S.op("act", lambda e: e.copy(out=xT[wb][:, :, blk * 128:(blk + 1) * 128], in_=tpe[ti][:]),
                 reads=[b_tpe[ti]], writes=[b_xT[wb][blk]])

        def e_st1(ex, fc):
            wb = ex % 2
            pi = (ex * 4 + fc) % 2
            for which, wt, bw in ((0, w1b, b_w1), (1, w3b, b_w3)):
                def _mm(e, wt=wt, bank=pi * 2 + which):
                    ins = None
                    for c in range(8):
                        ins = e.matmul(hps_[bank][:], lhsT=wt[wb][:, c, fc * 128:(fc + 1) * 128],
                                       rhs=xT[wb][:, c, :], start=(c == 0), stop=(c == 7))
                    return ins
                S.op("pe", _mm, reads=[bw[wb]] + b_xT[wb], writes=[b_hpsE[pi * 2 + which]])
            S.op("act", lambda e: e.activation(out=s1[pi][:], in_=hps_[pi * 2][:], func=AF.Silu),
                 reads=[b_hpsE[pi * 2]], writes=[b_s1[pi]])
            S.op("dve", lambda e: e.tensor_tensor(
                out=hT[wb][:, fc, :], in0=hps_[pi * 2 + 1][:], in1=s1[pi][:], op=ALU.mult),
                reads=[b_hpsE[pi * 2 + 1], b_s1[pi]], writes=[b_hT[wb][fc]])

        def e_st2(ex, blk):
            wb = ex % 2
            n = ex * 4 + blk
            yi = n % 4
            for half in range(2):
                def _ymm(e, half=half):
                    ins = None
                    for fc in range(4):
                        ins = e.matmul(yps[half][:], lhsT=hT[wb][:, fc, blk * 128:(blk + 1) * 128],
                                       rhs=w2b[ex % 3][:, fc, half * 512:(half + 1) * 512],
                                       start=(fc == 0), stop=(fc == 3))
                    return ins
                S.op("pe", _ymm, reads=b_hT[wb] + [b_w2[ex % 3]], writes=[b_yps[half]])
                S.op("dve", lambda e, half=half: e.tensor_copy(out=yb[yi][:, half * 512:(half + 1) * 512],
                                                               in_=yps[half][:]),
                     reads=[b_yps[half]], writes=[b_ybh[yi][half]])
            S.dma("pool", lambda e: e.dma_start(out=YS[n * 128:n * 128 + 128, :], in_=yb[yi][:]),
                  reads=b_ybh[yi], writes=[b_YS[n]])
        for s_ in range(NE + 2):
            for q in range(4):
                if 0 <= s_ - 1 < NE:
                    e_st1(s_ - 1, q)
                if 0 <= s_ - 2 < NE:
                    e_st2(s_ - 2, q)
                if s_ < NE:
                    e_st0(s_, q)
        S.barrier()
    out_bufs += b_YS
    if stop_after == "E":
        S.finish("sp", out_bufs)
        return nc, es

    b_out = bufs("out", NT)
    with ExitStack() as pc:
        pc.enter_context(nc.named_scope("phC"))
        wgb = sb(pc, "wgb", [128, 8, D], BF16)
        wpb = sb(pc, "wpb", [128, 2, D], BF16)
        gple = sb(pc, "gple", [128, 8], F32)
        gfin = sb(pc, "gfin", [128, D], F32)
        b_cc = [Buf(f"cconst{i}") for i in range(4)]
        wgst = [sb(pc, f"wgst{i}", [128, 4, D], F32) for i in range(2)]
        b_wgst = bufs("wgst", 2)
        S.dma("sp", lambda e: e.dma_start(out=gple[:], in_=gple_d[:, :]), writes=[b_cc[0]])
        S.dma("sp", lambda e: e.dma_start(out=gfin[:], in_=gfin_d.rearrange("a n -> (a n)").partition_broadcast(128)),
              writes=[b_cc[1]])
        S.dma("pool", lambda e: e.dma_start(out=wpb[:], in_=wproj_d.rearrange("(c p) n -> p c n", p=128)),
              writes=[b_cc[2]])
        wgv = wgate_d.rearrange("(c p) n -> p c n", p=128)
        for hh in range(2):
            S.dma("sp", lambda e, hh=hh: e.dma_start(out=wgst[hh][:], in_=wgv[:, hh * 4:(hh + 1) * 4, :]),
                  writes=[b_wgst[hh]])

            def _wg(e, hh=hh):
                ins = None
                for c4 in range(4):
                    c = hh * 4 + c4
                    ins = e.tensor_scalar(out=wgb[:, c, :], in0=wgst[hh][:, c4, :], scalar1=gple[:, c:c + 1],
                                          scalar2=None, op0=ALU.mult)
                return ins
            S.op("dve", _wg, reads=[b_wgst[hh], b_cc[0]], writes=[b_cc[3]])
        h1c = [sb(pc, f"h1c{i}", [128, D], F32) for i in range(4)]
        y0 = [sb(pc, f"y0_{i}", [128, D], F32) for i in range(4)]
        y1 = [sb(pc, f"y1_{i}", [128, D], F32) for i in range(4)]
        b_h1c = bufs("h1c", 4)
        b_y0 = bufs("y0_", 4)
        b_y1 = bufs("y1_", 4)
        NH2 = 5
        h2 = [sb(pc, f"h2_{i}", [128, D], F32) for i in range(NH2)]
        b_h2 = bufs("h2_", NH2)
        xn3 = [sb(pc, f"xn3_{i}", [128, D], BF16) for i in range(2)]
        b_xn3 = bufs("xn3_", 2)
        xn3T = [sb(pc, f"xn3T{i}", [128, 8, 128], BF16) for i in range(2)]
        b_xn3T = bufs("xn3T", 2)
        pt32 = [sb(pc, f"pt32_{i}", [128, PLE], F32) for i in range(4)]
        ptb = [sb(pc, f"ptb{i}", [128, PLE], BF16) for i in range(2)]
        pTT = [sb(pc, f"pTT{i}", [128, 2, 128], BF16) for i in range(2)]
        b_pt32 = bufs("pt32", 4)
        b_ptb = bufs("ptb", 2)
        b_pTT = bufs("pTT", 2)
        gsb = [sb(pc, f"gsb{i}", [128, D], F32) for i in range(2)]
        b_gsb = bufs("gsb", 2)
        tq = [sb(pc, f"tq{i}", [128, D], F32) for i in range(3)]
        b_tq = bufs("tq", 3)
        ot = [sb(pc, f"ot{i}", [128, D], F32) for i in range(2)]
        b_ot = bufs("ot", 2)
        junkc = sb(pc, "junkc", [128, D], BF16)
        b_junkc = Buf("junkc")
        stc = sb(pc, "stc", [128, NT, 12], F32)
        b_stc = bufs("stc", NT)
        b_stc2 = bufs("stc2_", NT)
        tpc = ps(pc, "tpc", [128, 8, 128], BF16)
        b_tpc = Buf("tpc")
        tpp = ps(pc, "tpp", [128, 8, 128], BF16)
        b_tpp = Buf("tpp")
        gps = [ps(pc, f"gps{i}", [128, 512], F32) for i in range(2)]
        b_gps = bufs("gps", 2)
        pps = [ps(pc, f"pps{i}", [128, 512], F32) for i in range(2)]
        b_pps = bufs("pps", 2)

        def c_m0(j):
            i4 = j % 4
            S.dma("sp", lambda e: e.dma_start(out=h1c[i4][:], in_=H1[j * 128:(j + 1) * 128, :]),
                  reads=[b_H1[j]], writes=[b_h1c[i4]])
            S.dma("sp", lambda e: e.dma_start(out=pt32[i4][:], in_=p_d[j * 128:(j + 1) * 128, :]),
                  writes=[b_pt32[i4]])
            for k, yy, byy in ((0, y0, b_y0), (1, y1, b_y1)):
                S.dma("pool", lambda e, k=k, yy=yy: e.indirect_dma_start(
                    out=yy[i4][:], out_offset=None, in_=YS[:, :],
                    in_offset=bass.IndirectOffsetOnAxis(ap=desti[:, j, k:k + 1], axis=0)),
                    reads=b_YS + [b_route[j]], writes=[byy[i4]])

        def c_m1(j):
            i4, i5 = j % 4, j % NH2
            S.op("dve", lambda e: e.scalar_tensor_tensor(
                out=h2[i5][:], in0=y0[i4][:], scalar=gates[:, j, 0:1], in1=h1c[i4][:], op0=ALU.mult, op1=ALU.add),
                reads=[b_y0[i4], b_h1c[i4], b_route[j]], writes=[b_h2[i5]])
            S.op("dve", lambda e: e.scalar_tensor_tensor(
                out=h2[i5][:], in0=y1[i4][:], scalar=gates[:, j, 1:2], in1=h2[i5][:], op0=ALU.mult, op1=ALU.add),
                reads=[b_y1[i4], b_h2[i5], b_route[j]], writes=[b_h2[i5]])
            S.op("act", lambda e: e.activation(out=junkc[:], in_=h2[i5][:], func=AF.Square,
                                               accum_out=stc[:, j, 0:1]),
                 reads=[b_h2[i5]], writes=[b_junkc, b_stc[j]])

        def c_m2(j):
            i4, i5, i2 = j % 4, j % NH2, j % 2
            rsqrt_dve(stc[:, j, 0:6], 1.0 / D, [b_stc[j]])
            S.op("dve", lambda e: e.tensor_scalar(out=xn3[i2][:], in0=h2[i5][:], scalar1=stc[:, j, 2:3],
                                                  scalar2=None, op0=ALU.mult),
                 reads=[b_h2[i5], b_stc[j]], writes=[b_xn3[i2]])
            S.op("act", lambda e: e.copy(out=ptb[i2][:], in_=pt32[i4][:]), reads=[b_pt32[i4]], writes=[b_ptb[i2]])

        def c_m3(j):
            i2 = j % 2

            def _tr(e):
                ins = None
                for c in range(8):
                    ins = e.transpose(out=tpc[:, c, :], in_=xn3[i2][:, c * 128:(c + 1) * 128], identity=ident_bf[:])
                return ins
            S.op("pe", _tr, reads=[b_xn3[i2], b_ident], writes=[b_tpc])
            S.op("act", lambda e: e.copy(out=xn3T[i2][:], in_=tpc[:]), reads=[b_tpc], writes=[b_xn3T[i2]])

            def _trp(e):
                ins = None
                for c in range(2):
                    ins = e.transpose(out=tpp[:, c, :], in_=ptb[i2][:, c * 128:(c + 1) * 128], identity=ident_bf[:])
                return ins
            S.op("pe", _trp, reads=[b_ptb[i2], b_ident], writes=[b_tpp])
            S.op("act", lambda e: e.copy(out=pTT[i2][:], in_=tpp[:, 0:2, :]), reads=[b_tpp], writes=[b_pTT[i2]])

        def c_m4(j):
            i2, i3 = j % 2, j % 3
            for half in range(2):
                def _gmm(e, half=half):
                    ins = None
                    for c in range(8):
                        ins = e.matmul(gps[half][:], lhsT=xn3T[i2][:, c, :], rhs=wgb[:, c, half * 512:(half + 1) * 512],
                                       start=(c == 0), stop=(c == 7))
                    return ins
                S.op("pe", _gmm, reads=[b_xn3T[i2], b_cc[3]], writes=[b_gps[half]])
                S.op("act", lambda e, half=half: e.activation(
                    out=gsb[i2][:, half * 512:(half + 1) * 512], in_=gps[half][:], func=AF.Sigmoid),
                    reads=[b_gps[half]], writes=[b_gsb[i2]])

                def _pmm(e, half=half):
                    ins = None
                    for c in range(2):
                        ins = e.matmul(pps[half][:], lhsT=pTT[i2][:, c, :], rhs=wpb[:, c, half * 512:(half + 1) * 512],
                                       start=(c == 0), stop=(c == 1))
                    return ins
                S.op("pe", _pmm, reads=[b_pTT[i2], b_cc[2]], writes=[b_pps[half]])
                S.op("dve", lambda e, half=half: e.tensor_tensor(
                    out=tq[i3][:, half * 512:(half + 1) * 512], in0=pps[half][:],
                    in1=gsb[i2][:, half * 512:(half + 1) * 512], op=ALU.mult),
                    reads=[b_pps[half], b_gsb[i2]], writes=[b_tq[i3]])

        def c_m5(j):
            i3, i5 = j % 3, j % NH2
            S.op("pool", lambda e: e.tensor_tensor(out=tq[i3][:], in0=tq[i3][:], in1=h2[i5][:], op=ALU.add),
                 reads=[b_tq[i3], b_h2[i5]], writes=[b_tq[i3]])
            S.op("act", lambda e: e.activation(out=junkc[:], in_=tq[i3][:], func=AF.Square,
                                               accum_out=stc[:, j, 6:7]),
                 reads=[b_tq[i3]], writes=[b_junkc, b_stc2[j]])

        def c_m6(j):
            i3, i2 = j % 3, j % 2
            rsqrt_dve(stc[:, j, 6:12], 1.0 / D, [b_stc2[j]])
            S.op("dve", lambda e: e.scalar_tensor_tensor(
                out=ot[i2][:], in0=tq[i3][:], scalar=stc[:, j, 8:9], in1=gfin[:], op0=ALU.mult, op1=ALU.mult),
                reads=[b_tq[i3], b_stc2[j], b_cc[1]], writes=[b_ot[i2]])
            S.dma("sp", lambda e: e.dma_start(out=out[j * 128:(j + 1) * 128, :], in_=ot[i2][:]),
                  reads=[b_ot[i2]], writes=[b_out[j]])
        pipeline(NT, [c_m0, c_m1, c_m2, c_m3, c_m4, c_m5, c_m6], [0, 2, 3, 4, 5, 6, 7])
        S.barrier()
    out_bufs += b_out

    S.finish("sp", out_bufs)
    return nc, es


def _prep_inputs(inputs):
    cst = _consts()
    x = np.asarray(inputs["x"], dtype=np.float32)
    shared = {
        "w_in": np.ascontiguousarray(inputs["w_in"][0]),
        "gmix_pc": _pc(inputs["g_mix"][0], 8),
        "conv_w": np.ascontiguousarray(inputs["hy_conv_w"][0]),
        "conv_b": np.ascontiguousarray(inputs["hy_conv_b"][0]).reshape(1, HYC),
        "ident_bf": cst["ident_bf"],
        "ident_f": cst["ident_f"],
        "zT": cst["zT"], "dec_f": cst["dec_f"], "dec_b": cst["dec_b"],
        "Cf": cst["Cf"], "Sf": cst["Sf"], "Ci": cst["Ci"], "Si": cst["Si"],
        "fw1": np.ascontiguousarray(inputs["hy_f_w1"][0]),
        "fw2": np.ascontiguousarray(inputs["hy_f_w2"][0]),
        "fw3": np.ascontiguousarray(inputs["hy_f_w3"][0]),
        "fcol": np.ascontiguousarray(np.stack([inputs["hy_f_freq1"][0], inputs["hy_f_b1"][0],
                                               inputs["hy_f_freq2"][0], inputs["hy_f_b2"][0]], axis=1)),
        "skip": np.ascontiguousarray(inputs["hy_skip"][0]).reshape(1, 2 * HYW),
        "ghy": np.ascontiguousarray(inputs["g_out_hy"][0]).reshape(1, HYW),
        "gna": np.ascontiguousarray(inputs["g_out_na"][0]).reshape(1, HYW),
        "w_out": np.ascontiguousarray(inputs["w_out"][0]),
        "gffn": np.ascontiguousarray(inputs["g_ffn"][0]).reshape(1, D),
        "wr_pc": np.ascontiguousarray(np.concatenate([inputs["router_wg"][0], inputs["router_we"][0]], axis=1)
                                      .reshape(8, 128, 36).transpose(1, 0, 2)),
        "br": np.ascontiguousarray(np.concatenate([inputs["router_bg"][0], inputs["router_be"][0]])).reshape(1, 36),
        "ust": np.triu(np.ones((128, 128), np.float32), 1),
        "ones_f": np.ones((128, 128), np.float32),
        "ecap": (np.arange(NE, dtype=np.float32) * CAP).reshape(1, NE),
        "exp_w1": np.ascontiguousarray(inputs["exp_w1"][0]),
        "exp_w3": np.ascontiguousarray(inputs["exp_w3"][0]),
        "exp_w2": np.ascontiguousarray(inputs["exp_w2"][0]),
        "gple_pc": _pc(inputs["g_ple"][0], 8),
        "w_gate": np.ascontiguousarray(inputs["w_ple_gate"][0]),
        "w_proj": np.ascontiguousarray(inputs["w_ple_proj"][0]),
        "gfin": np.ascontiguousarray(inputs["g_final"]).reshape(1, D),
        "nmask": _natten_consts().reshape(128, 5 * 640),
        "rpbT": _rpb_toeplitz(np.asarray(inputs["na_rpb"][0], dtype=np.float32)),
    }
    maps = []
    for c in range(NCORES):
        m = dict(shared)
        m["x"] = np.ascontiguousarray(x[c * NSEQ:(c + 1) * NSEQ].reshape(T, D))
        m["p"] = np.ascontiguousarray(np.asarray(inputs["p"][0, c * NSEQ:(c + 1) * NSEQ], dtype=np.float32).reshape(T, PLE))
        maps.append(m)
    return maps


def kernel(**inputs):
    nc, es = build_nc()
    maps = _prep_inputs(inputs)
    res = run_bass_kernel_spmd(nc, maps, core_ids=list(range(NCORES)))
    es.close()
    outs = [np.asarray(r["out"]).reshape(NSEQ, L, D) for r in res.results]
    return np.concatenate(outs, axis=0).astype(np.float32)
```
